# Optimizing a Trainium2 kernel written in Bass

```python
import jax, jax.numpy as jnp
from jax import lax
import numpy as np

D_MODEL = 1024
BATCH = 4
SEQ = 4096
DEPTH = 1

PLE_DIM = 256
CONV_WIDTH = D_MODEL // 2
CONV_HEADS = 8
CONV_K = 3
SGU_WIDTH = D_MODEL // 2
SGU_HEADS = 8
SGU_HEAD_DIM = SGU_WIDTH // SGU_HEADS
CHUNK = 128
MIX_WIDTH = CONV_WIDTH + SGU_WIDTH
IN_PROJ_WIDTH = 3 * CONV_WIDTH + 2 * SGU_WIDTH
N_GROUPS = 4
EXPERTS_PER_GROUP = 8
TOP_K_INNER = 2
D_EXPERT = D_MODEL // 2
EPS = 1e-6

kernel_name = "hymba_conv_sgu_hmoe_block"


def rmsnorm(x, g):
    xf = x.astype(jnp.float32)
    y = xf * lax.rsqrt(jnp.mean(xf * xf, axis=-1, keepdims=True) + EPS)
    return (y * g.astype(jnp.float32)).astype(x.dtype)


def layernorm(x, g):
    xf = x.astype(jnp.float32)
    mu = jnp.mean(xf, axis=-1, keepdims=True)
    xc = xf - mu
    y = xc * lax.rsqrt(jnp.mean(xc * xc, axis=-1, keepdims=True) + EPS)
    return (y * g.astype(jnp.float32)).astype(x.dtype)


def short_conv_mixer(b_gate, c_gate, x_in, w_conv):
    seq = x_in.shape[1]
    z = c_gate * x_in
    zp = jnp.pad(z, ((0, 0), (CONV_K - 1, 0), (0, 0)))
    conv = sum(zp[:, k:k + seq, :] * w_conv[k] for k in range(CONV_K))
    return b_gate * conv


def sgu_mixer(u, v, g_sgu, w_spatial, b_spatial):
    bsz, seq, _ = v.shape
    v = layernorm(v, g_sgu)
    vc = v.reshape(bsz, seq // CHUNK, CHUNK, SGU_HEADS, SGU_HEAD_DIM)
    causal = jnp.tril(jnp.ones((CHUNK, CHUNK), dtype=bool))
    ws = jnp.where(causal[None], w_spatial, jnp.zeros_like(w_spatial))
    mixed = jnp.einsum('hts,bcshd->bcthd', ws, vc) + b_spatial.T[None, None, :, :, None]
    return u * mixed.reshape(bsz, seq, SGU_WIDTH)


def hierarchical_moe(h, w_group, b_group, w_router, b_router, w_gate, w_up, w_down):
    bsz, seq, d = h.shape
    t = h.reshape(bsz * seq, d)
    g_logits = (t @ w_group).astype(jnp.float32) + b_group.astype(jnp.float32)
    g_prob = jax.nn.softmax(g_logits, axis=-1)
    g_w, g_idx = lax.top_k(g_prob, 1)
    e_logits = jnp.einsum('td,gde->tge', t, w_router).astype(jnp.float32) + b_router.astype(jnp.float32)
    e_sel = jnp.take_along_axis(e_logits, g_idx[:, :, None], axis=1)[:, 0]
    e_val, e_idx = lax.top_k(e_sel, TOP_K_INNER)
    e_w = jax.nn.softmax(e_val, axis=-1) * g_w
    inner = jnp.sum(jax.nn.one_hot(e_idx, EXPERTS_PER_GROUP, dtype=jnp.float32) * e_w[..., None], axis=1)
    comb = (jax.nn.one_hot(g_idx[:, 0], N_GROUPS, dtype=jnp.float32)[:, :, None]
            * inner[:, None, :]).astype(h.dtype)
    y = jnp.zeros_like(t)
    for g in range(N_GROUPS):
        a = jnp.einsum('td,edf->tef', t, w_gate[g])
        b = jnp.einsum('td,edf->tef', t, w_up[g])
        hid = jax.nn.silu(a) * b * comb[:, g, :, None]
        y = y + jnp.einsum('tef,efd->td', hid, w_down[g])
    return y.reshape(bsz, seq, d)


def setup_inputs(seed: int = 0) -> dict:
    key = jax.random.key(seed)
    ks = jax.random.split(key, 24)
    f32 = jnp.float32
    n = lambda k, shape, scale: jax.random.normal(k, shape, f32) * scale
    gain = lambda k, shape: 1.0 + 0.05 * jax.random.normal(k, shape, f32)
    L, G, E = DEPTH, N_GROUPS, EXPERTS_PER_GROUP
    return {
        "x": n(ks[0], (BATCH, SEQ, D_MODEL), 1.0),
        "p": n(ks[1], (DEPTH, BATCH, SEQ, PLE_DIM), 1.0),
        "g_mix": gain(ks[2], (L, D_MODEL)),
        "w_in": n(ks[3], (L, D_MODEL, IN_PROJ_WIDTH), D_MODEL ** -0.5),
        "w_conv": n(ks[4], (L, CONV_K, CONV_WIDTH), CONV_K ** -0.5),
        "g_sgu": gain(ks[5], (L, SGU_WIDTH)),
        "w_spatial": n(ks[6], (L, SGU_HEADS, CHUNK, CHUNK), CHUNK ** -0.5),
        "b_spatial": 1.0 + n(ks[7], (L, SGU_HEADS, CHUNK), 0.1),
        "w_out": n(ks[8], (L, MIX_WIDTH, D_MODEL), MIX_WIDTH ** -0.5),
        "g_ffn": gain(ks[9], (L, D_MODEL)),
        "w_group": n(ks[10], (L, D_MODEL, G), D_MODEL ** -0.5),
        "b_group": n(ks[11], (L, G), 0.01),
        "w_router": n(ks[12], (L, G, D_MODEL, E), D_MODEL ** -0.5),
        "b_router": n(ks[13], (L, G, E), 0.01),
        "w_gate": n(ks[14], (L, G, E, D_MODEL, D_EXPERT), D_MODEL ** -0.5),
        "w_up": n(ks[15], (L, G, E, D_MODEL, D_EXPERT), D_MODEL ** -0.5),
        "w_down": n(ks[16], (L, G, E, D_EXPERT, D_MODEL), D_EXPERT ** -0.5),
        "g_ple": gain(ks[17], (L, D_MODEL)),
        "w_ple_gate": n(ks[18], (L, D_MODEL, D_MODEL), D_MODEL ** -0.5),
        "w_ple_proj": n(ks[19], (L, PLE_DIM, D_MODEL), PLE_DIM ** -0.5),
        "g_final": gain(ks[20], (D_MODEL,)),
    }


def reference(x, p, g_mix, w_in, w_conv, g_sgu, w_spatial, b_spatial, w_out, g_ffn,
              w_group, b_group, w_router, b_router, w_gate, w_up, w_down,
              g_ple, w_ple_gate, w_ple_proj, g_final):
    splits = [CONV_WIDTH, 2 * CONV_WIDTH, 3 * CONV_WIDTH, 3 * CONV_WIDTH + SGU_WIDTH]
    for i in range(DEPTH):
        h = rmsnorm(x, g_mix[i])
        z = h @ w_in[i]
        b_gate, c_gate, x_in, u, v = jnp.split(z, splits, axis=-1)
        y_conv = short_conv_mixer(b_gate, c_gate, x_in, w_conv[i])
        y_sgu = sgu_mixer(jax.nn.gelu(u), jax.nn.gelu(v), g_sgu[i],
                          w_spatial[i], b_spatial[i])
        x = x + jnp.concatenate([y_conv, y_sgu], axis=-1) @ w_out[i]
        x = x + hierarchical_moe(rmsnorm(x, g_ffn[i]), w_group[i], b_group[i], w_router[i],
                                 b_router[i], w_gate[i], w_up[i], w_down[i])
        gate = jax.nn.sigmoid(rmsnorm(x, g_ple[i]) @ w_ple_gate[i])
        x = x + gate * (p[i] @ w_ple_proj[i])
    return rmsnorm(x, g_final)
```

```python
import numpy as np
from contextlib import ExitStack

import concourse.bass as bass
import concourse.mybir as mybir
from concourse.bass_utils import run_bass_kernel_spmd

F32 = mybir.dt.float32
BF16 = mybir.dt.bfloat16
I32 = mybir.dt.int32
AF = mybir.ActivationFunctionType
ALU = mybir.AluOpType
AX = mybir.AxisListType

NCORES = 8
TOK = 2048
NT = 16
D = 1024
KD = 8
NE = 32
CAP = 384
NS = CAP // 128
FE = 512
EPS = 1e-6
BIG = 1.0e30
DEBUG = False
SYNC_ALL = True

ENGS = ("sync", "scalar", "vector", "gpsimd", "tensor")


class Sem:
    def __init__(self, h, step):
        self.h = h
        self.step = step
        self.n = 0
        self.last = None


class Buf:
    __slots__ = ("name", "w", "pw", "r")

    def __init__(self, name):
        self.name = name
        self.w = None
        self.pw = []
        self.r = []


class Op:
    __slots__ = ("eng", "fn", "dma", "target", "signal", "count", "deps", "odeps", "cidx", "pos", "strict",
                 "cost", "nbytes", "nleft", "kids", "okids", "ready", "fin", "iss")


DEF_COST = {"sync": 60.0, "scalar": 300.0, "vector": 250.0, "gpsimd": 500.0, "tensor": 300.0}
DMA_BW = 0.33e3
DMA_LAT = 2000.0


class _Dummy:
    def then_inc(self, *a, **k):
        return self


def _nfree(ap):
    n = 1
    for d in ap.shape[1:]:
        n *= int(d)
    return n


class CostProxy:
    def __init__(self, eng):
        self.eng = eng
        self.cost = 0.0
        self.nbytes = 0

    def to_reg(self, v):
        return None

    def __getattr__(self, name):
        def f(*args, **kw):
            out = kw.get("out", args[0] if args else None)
            eng = self.eng
            if name in ("dma_start", "indirect_dma_start"):
                src = kw.get("in_", args[1] if len(args) > 1 else out)
                n = min(_nfree(out) * int(out.shape[0]), _nfree(src) * int(src.shape[0]))
                self.nbytes += n * (2 if src.dtype == BF16 else 4)
                self.cost += {"sync": 80.0, "scalar": 80.0}.get(eng, 2500.0 if name.startswith("ind") else 1200.0)
            elif eng == "tensor":
                if name == "matmul":
                    rhs = kw.get("rhs", args[2] if len(args) > 2 else None)
                    self.cost += max(_nfree(rhs), 64) / 2.0 + 6.0
                else:
                    self.cost += 70.0
            elif eng == "scalar":
                self.cost += 224.0 + 0.833 * _nfree(out) + (60.0 if kw.get("accum_out") is not None else 0.0)
            elif eng == "vector":
                src = kw.get("in_", kw.get("in0", out))
                self.cost += 70.0 + 1.04 * max(_nfree(out), _nfree(src))
            else:
                self.cost += 300.0 + 2.0 * _nfree(out)
            return _Dummy()
        return f


class Prog:
    def __init__(self, nc, es):
        self.nc = nc
        self.es = es
        self.ops = []
        self.q = {e: [] for e in ENGS}
        self.esem = {e: Sem(es.enter_context(nc.semaphore("eng_" + e)), 1)
                     for e in ("scalar", "vector", "gpsimd", "tensor")}
        self.fence = {e: None for e in ENGS}
        self.since_fence = []

    def dsem(self, name):
        return Sem(self.es.enter_context(self.nc.semaphore(name)), 16)

    def _add(self, eng, fn, deps, dma, strict=False, cost=None, nbytes=0):
        o = Op()
        o.eng = eng
        o.fn = fn
        o.dma = dma
        o.signal = False
        o.count = None
        o.target = None
        o.strict = strict
        if cost is None and fn is not None:
            px = CostProxy(eng)
            fn(px)
            cost, nbytes = px.cost, px.nbytes
        o.cost = DEF_COST[eng] if cost is None else float(cost)
        o.nbytes = nbytes
        deps = list(deps)
        o.odeps = []
        if dma is not None:
            dma.n += 16
            o.target = dma.n
            if dma.last is not None:
                assert dma.last.eng == eng
                o.odeps.append(dma.last)
            dma.last = o
        if self.fence[eng] is not None:
            deps.append(self.fence[eng])
        seen = set()
        o.deps = []
        for d in deps:
            if id(d) not in seen:
                seen.add(id(d))
                o.deps.append(d)
        o.cidx = len(self.ops)
        self.ops.append(o)
        self.since_fence.append(o)
        return o

    def op(self, eng, fn, reads=(), writes=(), pwrites=(), dma=None, strict=False, cost=None, nbytes=0, bscale=1.0):
        deps = []
        for b in reads:
            if b.w is not None:
                deps.append(b.w)
            deps.extend(b.pw)
        for b in tuple(writes) + tuple(pwrites):
            if b.w is not None:
                deps.append(b.w)
            deps.extend(b.r)
        for b in writes:
            deps.extend(b.pw)
        o = self._add(eng, fn, deps, dma, strict, cost, nbytes)
        o.nbytes = o.nbytes * bscale
        for b in writes:
            b.w = o
            b.pw = []
            b.r = []
        for b in pwrites:
            b.pw.append(o)
        for b in reads:
            if b not in writes and b not in pwrites:
                b.r.append(o)
        return o

    def barrier(self):
        prev = list(self.since_fence)
        fs = {}
        for e in ENGS:
            fs[e] = self._add(e, None, prev, None, cost=0.0)
        self.fence = fs
        self.since_fence = []

    def wait_all(self, eng, ops):
        self._add(eng, None, list(ops), None, cost=0.0)

    def schedule(self):
        for o in self.ops:
            o.nleft = len(o.deps) + len(o.odeps)
            o.kids = []
            o.okids = []
            o.ready = 0.0
            o.fin = None
        for o in self.ops:
            for d in o.deps:
                d.kids.append(o)
            for d in o.odeps:
                d.okids.append(o)
        avail = {e: [] for e in ENGS}
        for o in self.ops:
            if o.nleft == 0:
                avail[o.eng].append(o)
        free = {e: 0.0 for e in ENGS}
        dma_free = 0.0
        n_left = len(self.ops)
        LOOK = 24
        SLACK = 400.0
        while n_left:
            best = None
            for e in ENGS:
                av = avail[e]
                if not av:
                    continue
                av.sort(key=lambda o: o.cidx)
                sts = [(max(free[e], o.ready), o) for o in av[:LOOK]]
                best_st = min(t for t, _ in sts)
                cand = None
                for t, o in sts:
                    if t <= best_st + SLACK:
                        cand = (t, o)
                        break
                if best is None or cand[0] < best[0] - 1e-9 or (abs(cand[0] - best[0]) <= 1e-9 and cand[1].cidx < best[1].cidx):
                    best = cand
            st, o = best
            e = o.eng
            avail[e].remove(o)
            free[e] = st + o.cost
            if o.dma is not None:
                dma_free = max(dma_free, st) + o.nbytes / DMA_BW
                o.fin = max(st + DMA_LAT, dma_free)
            else:
                o.fin = st + o.cost
            o.pos = len(self.q[e])
            self.q[e].append(o)
            n_left -= 1
            for k in o.kids:
                lat = 0.0
                if k.eng == o.eng and o.dma is None and k.dma is None and o.fn is not None:
                    lat = 1500.0 if k.strict else (400.0 if SYNC_ALL else 0.0)
                k.ready = max(k.ready, o.fin + lat)
                k.nleft -= 1
                if k.nleft == 0:
                    avail[k.eng].append(k)
            for k in o.okids:
                k.ready = max(k.ready, st + o.cost)
                k.nleft -= 1
                if k.nleft == 0:
                    avail[k.eng].append(k)
        self.makespan = max(o.fin for o in self.ops)

    def emit(self, block):
        self.schedule()
        for e in ENGS:
            for o in self.q[e]:
                best = {}
                for d in o.deps:
                    if d.fn is None and d.dma is None:
                        assert d.eng == o.eng
                        continue
                    if d.dma is None and o.dma is None and d.eng == o.eng and not (o.strict or SYNC_ALL):
                        assert d.pos < o.pos
                        continue
                    if d.dma is not None:
                        k = ("d", id(d.dma))
                        if k not in best or d.target > best[k].target:
                            best[k] = d
                    else:
                        k = ("e", d.eng)
                        if k not in best or d.pos > best[k].pos:
                            best[k] = d
                for d in o.odeps:
                    assert d.eng == o.eng and d.pos < o.pos
                o.deps = list(best.values())
                for d in o.deps:
                    if d.dma is None:
                        d.signal = True
        for e in ("scalar", "vector", "gpsimd", "tensor"):
            c = 0
            for o in self.q[e]:
                if o.dma is None and o.signal:
                    assert o.fn is not None
                    c += 1
                    o.count = c
        prog = self

        def run(eobj, ename):
            waited = {}
            for o in prog.q[ename]:
                for d in o.deps:
                    if d.dma is not None:
                        sem, val = d.dma.h, d.target
                    else:
                        sem, val = prog.esem[d.eng].h, d.count
                    k = id(sem)
                    if waited.get(k, 0) >= val:
                        continue
                    eobj.wait_ge(sem, val)
                    waited[k] = val
                if o.fn is None:
                    continue
                ins = o.fn(eobj)
                if o.dma is not None:
                    ins.then_inc(o.dma.h, 16)
                elif o.signal:
                    ins.then_inc(prog.esem[ename].h, 1)

        @block.sync
        def _(e):
            run(e, "sync")

        @block.scalar
        def _(e):
            run(e, "scalar")

        @block.vector
        def _(e):
            run(e, "vector")

        @block.gpsimd
        def _(e):
            run(e, "gpsimd")

        @block.tensor
        def _(e):
            run(e, "tensor")


def build_nc():
    nc = bass.Bass("TRN2", target_bir_lowering=False)

    def din(name, shape):
        return nc.dram_tensor(name, list(shape), F32, kind="ExternalInput").ap()

    x_d = din("x", [TOK, D])
    xh_d = din("xh", [2, D])
    p_d = din("p", [TOK, 256])
    gmix_d = din("g_mix", [1, D])
    win_d = din("w_in", [D, 2560])
    wconv_d = din("w_conv", [3, 512])
    gsgu_d = din("g_sgu", [1, 512])
    wsp_d = din("w_spatial", [8, 128, 128])
    bsp_d = din("b_spatial", [8, 128])
    wout_d = din("w_out", [D, D])
    gffn_d = din("g_ffn", [1, D])
    wgrp_d = din("w_group", [D, 4])
    bgrp_d = din("b_group", [1, 4])
    wrt_d = din("w_router", [4, D, 8])
    brt_d = din("b_router", [1, 32])
    wg_d = din("w_gate", [NE, D, FE])
    wu_d = din("w_up", [NE, D, FE])
    wd_d = din("w_down", [NE, FE, D])
    gple_d = din("g_ple", [1, D])
    wpg_d = din("w_ple_gate", [D, D])
    wpp_d = din("w_ple_proj", [256, D])
    gfin_d = din("g_final", [1, D])
    out_d = nc.dram_tensor("out", [TOK, D], F32, kind="ExternalOutput").ap()
    h2buf_d = nc.dram_tensor("h2buf", [TOK, D], BF16, kind="Internal").ap()
    xbuf_d = nc.dram_tensor("xbuf", [2 * TOK, D], BF16, kind="Internal").ap()
    ybuf_d = nc.dram_tensor("ybuf", [2 * TOK, D], F32, kind="Internal").ap()

    if DEBUG:
        dbg_x2 = nc.dram_tensor("dbg_x2", [TOK, D], F32, kind="ExternalOutput").ap()
        dbg_x3 = nc.dram_tensor("dbg_x3", [TOK, D], F32, kind="ExternalOutput").ap()
        dbg_L = nc.dram_tensor("dbg_L", [128, NT, 36], F32, kind="ExternalOutput").ap()
        dbg_i1 = nc.dram_tensor("dbg_i1", [128, NT], I32, kind="ExternalOutput").ap()
        dbg_i2 = nc.dram_tensor("dbg_i2", [128, NT], I32, kind="ExternalOutput").ap()
        dbg_w1 = nc.dram_tensor("dbg_w1", [128, NT], F32, kind="ExternalOutput").ap()
        dbg_w2 = nc.dram_tensor("dbg_w2", [128, NT], F32, kind="ExternalOutput").ap()
        dbg_yT = nc.dram_tensor("dbg_yT", [4, 128, KD, 512], BF16, kind="ExternalOutput").ap()

    with ExitStack() as es:
        def sb(name, shape, dt):
            return es.enter_context(nc.sbuf_tensor(name, list(shape), dt))

        P = Prog(nc, es)
        ps = es.enter_context(nc.psum_tensor("ps", [128, 8, 512], F32))
        bank = [Buf("bank%d" % i) for i in range(8)]

        def bank_bf(b, k):
            return ps[:, b, 0:64 * k].bitcast(BF16).rearrange("p (k t) -> p k t", k=k)

        xres = sb("xres", [128, NT, D], F32)
        xresB = [Buf("xres%d" % j) for j in range(NT)]
        gA = sb("gA", [128, D], F32)
        gB_ = sb("gB", [128, D], F32)
        gAB, gBB = Buf("gA"), Buf("gB")
        ident = sb("ident", [128, 128], BF16)
        ltri = sb("ltri", [128, 128], BF16)
        ones_bf = sb("ones_bf", [128, 128], BF16)
        onesf = sb("onesf", [128, 512], F32)
        mhalf = sb("mhalf", [128, 1], F32)
        gplec = sb("gplec", [128, KD], F32)
        gplecB = Buf("gplec")
        tickB = [[Buf("tick%d_%d" % (b, h)) for h in range(4)] for b in range(4)]
        constB = Buf("const")
        identB = Buf("ident")
        ss = sb("ss", [128, 8, NT], F32)
        _ssB = {}

        def ssb(row, j):
            if (row, j) not in _ssB:
                _ssB[(row, j)] = Buf("ss%d_%d" % (row, j))
            return _ssB[(row, j)]
        idx1 = sb("idx1", [128, NT], I32)
        idx2 = sb("idx2", [128, NT], I32)
        w1g = sb("w1g", [128, NT], F32)
        w2g = sb("w2g", [128, NT], F32)
        routeB = Buf("route")
        sidx_i = sb("sidx_i", [128, NE, NS], I32)
        sidxB = Buf("sidx")
        ARENA = 127 * 1024
        arena = sb("arena", [128, ARENA // 2], BF16)
        apos = [0]

        def carve(shape, dt):
            n = int(np.prod(shape[1:]))
            nb = n * (4 if dt in (F32, I32) else 2)
            nb = (nb + 31) // 32 * 32
            a = apos[0]
            assert a + nb <= ARENA, "arena overflow %d" % (a + nb)
            apos[0] = a + nb
            v = arena[0:shape[0], a // 2:(a + nb) // 2]
            if dt != BF16:
                v = v.bitcast(dt)
            v = v[:, 0:n]
            if len(shape) == 3:
                v = v.rearrange("p (a b) -> p a b", a=shape[1])
            elif len(shape) == 4:
                v = v.rearrange("p (a b c) -> p a b c", a=shape[1], b=shape[2])
            return v

        win_bf = [carve([128, KD, 512], BF16) for _ in range(5)]
        winB = [Buf("win%d" % i) for i in range(5)]
        wout_bf = carve([128, KD, D], BF16)
        woutB = Buf("wout")
        hT = carve([128, KD, 512], BF16)
        hTB = [Buf("hT%d" % i) for i in range(4)]
        htok = [carve([128, D], BF16) for _ in range(2)]
        htokB = [Buf("htok%d" % i) for i in range(2)]
        yT = carve([128, KD, 512], BF16)
        yTcB = [Buf("yTc%d" % i) for i in range(4)]
        yTsB = [Buf("yTs%d" % i) for i in range(4)]
        c_sb = [carve([128, 512], F32) for _ in range(2)]
        b_sb = [carve([128, 512], F32) for _ in range(2)]
        c_sbB = [Buf("c_sb%d" % i) for i in range(2)]
        b_sbB = [Buf("b_sb%d" % i) for i in range(2)]
        zc = carve([128, 4, 514], F32)
        zcB = [Buf("zc%d" % i) for i in range(4)]
        acc = [carve([128, 512], F32) for _ in range(2)]
        accB = [Buf("acc%d" % i) for i in range(2)]
        gu = [carve([128, 512], F32) for _ in range(2)]
        gv = [carve([128, 512], F32) for _ in range(2)]
        tmpv = carve([128, 512], F32)
        guB = [Buf("gu%d" % i) for i in range(2)]
        gvB = [Buf("gv%d" % i) for i in range(2)]
        tmpvB = Buf("tmpv")
        vn = carve([128, 512], BF16)
        ysg = carve([128, 512], BF16)
        vnB, ysgB = Buf("vn"), Buf("ysg")
        a0 = apos[0]
        xh_t = carve([128, D], F32)
        xhB = Buf("xh")
        hTh = carve([128, KD, 128], BF16)
        hThB = Buf("hTh")
        wsp_bf = carve([128, 8, 128], BF16)
        a1 = apos[0]
        apos[0] = a0
        h2t = [carve([128, D], BF16) for _ in range(2)]
        h2tB = [Buf("h2t%d" % i) for i in range(2)]
        h2T = [carve([128, KD, 128], BF16) for _ in range(2)]
        h2TB = [Buf("h2T%d" % i) for i in range(2)]
        apos[0] = max(apos[0], a1)
        wsT = carve([128, 8, 128], BF16)
        wspB, wsTB = Buf("wsp"), Buf("wsT")
        gsguB_t = carve([128, 512], F32)
        gsguB = Buf("gsgu")
        biasB_t = carve([128, 36], F32)
        biasB = Buf("bias")
        wconvT = carve([128, 4, 3], F32)
        wconvB = Buf("wconv")
        R_f = tmpv[0:8, :]
        R_bf = carve([8, 512], BF16)
        RB = Buf("R")
        bsp_f = carve([8, 128], F32)
        bsp_t = carve([8, 128], F32)
        bhi = carve([8, 128], BF16)
        blo = carve([8, 128], BF16)
        bspB = Buf("bsp")
        bsp4B = Buf("bsp4")
        wr_bf = carve([128, KD, 36], BF16)
        wrB = Buf("wr")
        mixer_end = apos[0]
        L_BYTES = NT * 36 * 4
        L_start = ARENA - L_BYTES
        assert mixer_end <= L_start, "mixer arena overflow %d" % mixer_end
        apos[0] = L_start
        L = carve([128, NT, 36], F32)
        LB = Buf("L")

        h2bufB = [Buf("h2buf%d" % j) for j in range(NT)]
        xbufB = Buf("xbuf")
        ybufB = Buf("ybuf")

        s_x = [P.dsem("s_x%d" % b) for b in range(4)]

        def load_x(b, after=()):
            P.op("sync", lambda e, b=b: e.dma_start(
                out=xres[:, 4 * b:4 * b + 4, :],
                in_=x_d[512 * b:512 * (b + 1), :].rearrange("(j p) d -> p j d", p=128)),
                reads=list(after), writes=xresB[4 * b:4 * b + 4], dma=s_x[b])
        load_x(0)
        s_c = [P.dsem("s_c%d" % i) for i in range(12)]
        P.op("sync", lambda e: e.dma_start(out=gA[:], in_=gmix_d.partition_broadcast(128)),
             writes=[gAB], dma=s_c[0])
        P.op("sync", lambda e: e.dma_start(out=gB_[:], in_=gffn_d.partition_broadcast(128)),
             writes=[gBB], dma=s_c[1])
        P.op("sync", lambda e: e.dma_start(out=gsguB_t, in_=gsgu_d.partition_broadcast(128)),
             writes=[gsguB], dma=s_c[2])
        P.op("sync", lambda e: e.dma_start(out=biasB_t[:, 0:4], in_=bgrp_d.partition_broadcast(128)),
             pwrites=[biasB], dma=s_c[3])
        P.op("sync", lambda e: e.dma_start(out=biasB_t[:, 4:36], in_=brt_d.partition_broadcast(128)),
             pwrites=[biasB], dma=s_c[3])

        def ld_wconv(e):
            with nc.allow_non_contiguous_dma(reason="tiny conv taps"):
                n = 0
                for k in range(3):
                    for fc in range(4):
                        i = e.dma_start(out=wconvT[:, fc, k:k + 1],
                                        in_=wconv_d[k:k + 1, 128 * fc:128 * (fc + 1)].rearrange("o p -> p o"))
                        n += 1
                        if n < 12:
                            i.then_inc(s_c[4].h, 16)
                return i
        s_c[4].n += 16 * 11
        P.op("sync", ld_wconv, writes=[wconvB], dma=s_c[4])
        P.op("sync", lambda e: e.dma_start(out=bsp_f, in_=bsp_d), writes=[bspB], dma=s_c[5])

        P.op("gpsimd", lambda e: e.memset(onesf[:], 1.0), writes=[constB])
        P.op("gpsimd", lambda e: e.memset(mhalf[:], -0.5), pwrites=[constB])


        P.op("gpsimd", lambda e: e.memset(ones_bf[:], 1.0), pwrites=[constB])
        P.op("gpsimd", lambda e: e.affine_select(out=ident[:], in_=onesf[:, 0:128], pattern=[[1, 128]],
                                                 compare_op=ALU.is_equal, fill=0.0, base=0, channel_multiplier=-1),
             reads=[constB], pwrites=[identB])
        P.op("gpsimd", lambda e: e.affine_select(out=ltri[:], in_=onesf[:, 0:128], pattern=[[1, 128]],
                                                 compare_op=ALU.is_gt, fill=0.0, base=0, channel_multiplier=-1),
             reads=[constB], pwrites=[identB])

        P.op("gpsimd", lambda e: e.affine_select(out=R_f, in_=onesf[0:8, :], pattern=[[1, 512]],
                                                 compare_op=ALU.is_ge, fill=0.0, base=0, channel_multiplier=-64),
             reads=[constB], writes=[tmpvB])
        P.op("gpsimd", lambda e: e.affine_select(out=R_bf, in_=R_f, pattern=[[-1, 512]],
                                                 compare_op=ALU.is_ge, fill=0.0, base=63, channel_multiplier=64),
             reads=[tmpvB], writes=[RB])

        s_win = [P.dsem("s_win%d" % i) for i in range(5)]
        s_w0 = [P.dsem("s_w0_%d" % i) for i in range(4)]
        P.op("gpsimd", lambda e: e.dma_start(out=wsp_bf, in_=wsp_d.rearrange("h t s -> t h s")),
             writes=[wspB], dma=s_w0[0])
        for cg in (1, 2, 0, 3, 4):
            P.op("gpsimd", lambda e, cg=cg: e.dma_start(
                out=win_bf[cg], in_=win_d[:, 512 * cg:512 * (cg + 1)].rearrange("(k p) f -> p k f", p=128)),
                writes=[winB[cg]], dma=s_win[cg])
        P.op("gpsimd", lambda e: e.dma_start(out=wout_bf, in_=wout_d.rearrange("(k p) f -> p k f", p=128)),
             writes=[woutB], dma=s_w0[1])

        def ld_wr(e):
            with nc.allow_non_contiguous_dma(reason="router weights"):
                e.dma_start(out=wr_bf[:, :, 0:4], in_=wgrp_d.rearrange("(k p) e -> p k e", p=128)).then_inc(s_w0[2].h, 16)
                for g in range(3):
                    e.dma_start(out=wr_bf[:, :, 4 + 8 * g:12 + 8 * g],
                                in_=wrt_d[g].rearrange("(k p) e -> p k e", p=128)).then_inc(s_w0[2].h, 16)
                return e.dma_start(out=wr_bf[:, :, 28:36], in_=wrt_d[3].rearrange("(k p) e -> p k e", p=128))
        s_w0[2].n += 64
        P.op("gpsimd", ld_wr, writes=[wrB], dma=s_w0[2])

        bsp2B, bsp3B = Buf("bsp2"), Buf("bsp3")
        P.op("vector", lambda e: e.tensor_copy(out=bhi, in_=bsp_f), reads=[bspB], writes=[bsp2B])
        P.op("vector", lambda e: e.tensor_tensor(out=bsp_t, in0=bsp_f, in1=bhi, op=ALU.subtract), reads=[bspB, bsp2B], writes=[bsp3B])
        P.op("vector", lambda e: e.tensor_copy(out=blo, in_=bsp_t), reads=[bsp3B], writes=[bsp4B])

        def tr_ws(e):
            for h in range(8):
                i = e.transpose(out=bank_bf(0, 8)[:, h, :], in_=wsp_bf[:, h, :], identity=ident[:])
            return i
        P.op("tensor", tr_ws, reads=[wspB, identB], writes=[bank[0]])
        P.op("vector", lambda e: e.tensor_copy(out=wsT, in_=bank_bf(0, 8)), reads=[bank[0]], writes=[wsTB])
        P.op("gpsimd", lambda e: e.affine_select(out=wsT, in_=wsT, pattern=[[0, 8], [1, 128]],
                                                 compare_op=ALU.is_ge, fill=0.0, base=0, channel_multiplier=-1),
             writes=[wsTB])

        _regs = {}

        def bc_reg(e):
            if isinstance(e, CostProxy):
                return None
            if "bc" not in _regs:
                _regs["bc"] = e.to_reg(2 * TOK - 1)
            return _regs["bc"]

        def rstd2(col_ss, col_out, n, rb, wb, eps=EPS):
            P.op("gpsimd", lambda e: e.tensor_scalar(out=col_out, in0=col_ss, scalar1=1.0 / n, scalar2=eps,
                                                     op0=ALU.mult, op1=ALU.add), reads=[rb], writes=[wb])
            P.op("gpsimd", lambda e: e.tensor_tensor(out=col_out, in0=col_out, in1=mhalf[:], op=ALU.pow),
                 reads=[constB], writes=[wb])

        P.op("gpsimd", lambda e: e.memset(xh_t, 0.0), writes=[xhB])
        s_xh = P.dsem("s_xh")
        P.op("sync", lambda e: e.dma_start(out=xh_t[0:2, :], in_=xh_d), writes=[xhB], dma=s_xh)
        P.op("scalar", lambda e: e.activation(out=htok[0], in_=xh_t, func=AF.Square, accum_out=ss[:, 7, 0:1]),
             reads=[xhB], writes=[htokB[0], ssb(7, 0)])
        rstd2(ss[:, 7, 0:1], ss[:, 7, 1:2], D, ssb(7, 0), ssb(7, 1))
        P.op("vector", lambda e: e.scalar_tensor_tensor(out=htok[0], in0=xh_t, scalar=ss[:, 7, 1:2], in1=gA[:],
                                                        op0=ALU.mult, op1=ALU.mult),
             reads=[xhB, ssb(7, 1), gAB], writes=[htokB[0]])

        def tr_generic(src, nb, k):
            def f(e):
                for kk in range(k):
                    i = e.transpose(out=bank_bf(nb, k)[:, kk, :], in_=src[:, 128 * kk:128 * (kk + 1)], identity=ident[:])
                return i
            return f
        P.op("tensor", tr_generic(htok[0], 0, 8), reads=[htokB[0], identB], writes=[bank[0]])
        P.op("scalar", lambda e: e.copy(out=hTh, in_=bank_bf(0, 8)), reads=[bank[0]], writes=[hThB])

        def mm_halo(e):
            for gi, cg in enumerate((1, 2)):
                for fc in range(4):
                    for k in range(KD):
                        i = e.matmul(ps[:, 1, (gi * 4 + fc) * 2:(gi * 4 + fc) * 2 + 2],
                                     lhsT=win_bf[cg][:, k, 128 * fc:128 * (fc + 1)], rhs=hTh[:, k, 0:2],
                                     start=(k == 0), stop=(k == KD - 1))
            return i
        P.op("tensor", mm_halo, reads=[hThB, winB[1], winB[2]], writes=[bank[1]])
        halo_c = ss[:, 7, 8:16].rearrange("p (f t) -> p f t", f=4)
        P.op("vector", lambda e: e.tensor_copy(out=halo_c, in_=ps[:, 1, 0:8].rearrange("p (f t) -> p f t", f=4)),
             reads=[bank[1]], writes=[ssb(6, 0)])
        P.op("vector", lambda e: e.tensor_tensor(out=zc[:, :, 0:2], in0=halo_c,
                                                 in1=ps[:, 1, 8:16].rearrange("p (f t) -> p f t", f=4), op=ALU.mult),
             reads=[bank[1], ssb(6, 0)], writes=zcB, strict=True)

        s_h2 = [P.dsem("s_h2_%d" % i) for i in range(2)]
        s_dbg = P.dsem("s_dbg")
        for blk in range(4):
            for jl in range(4):
                j = 4 * blk + jl
                P.op("scalar", lambda e, j=j: e.activation(out=htok[j % 2], in_=xres[:, j, :], func=AF.Square,
                                                           accum_out=ss[:, 0, j:j + 1]),
                     reads=[xresB[j]], writes=[htokB[j % 2], ssb(0, j)])
                rstd2(ss[:, 0, j:j + 1], ss[:, 1, j:j + 1], D, ssb(0, j), ssb(1, j))
                P.op("vector", lambda e, j=j: e.scalar_tensor_tensor(
                    out=htok[j % 2], in0=xres[:, j, :], scalar=ss[:, 1, j:j + 1], in1=gA[:],
                    op0=ALU.mult, op1=ALU.mult), reads=[xresB[j], ssb(1, j), gAB], writes=[htokB[j % 2]])
                P.op("tensor", tr_generic(htok[j % 2], 0, 8), reads=[htokB[j % 2], identB], writes=[bank[0]])
                P.op("scalar", lambda e, jl=jl: e.copy(out=hT[:, :, 128 * jl:128 * (jl + 1)], in_=bank_bf(0, 8)),
                     reads=[bank[0]], writes=[hTB[jl], tickB[blk][jl]])
            if blk < 3:
                load_x(blk + 1, after=[tickB[blk][0]])
            for fc in range(4):
                def mm_conv(e, fc=fc):
                    for bi, cg in ((2, 0), (3, 1), (4, 2)):
                        for k in range(KD):
                            i = e.matmul(ps[:, bi, :], lhsT=win_bf[cg][:, k, 128 * fc:128 * (fc + 1)],
                                         rhs=hT[:, k, :], start=(k == 0), stop=(k == KD - 1))
                    return i
                P.op("tensor", mm_conv, reads=hTB + [winB[0], winB[1], winB[2]], writes=[bank[2], bank[3], bank[4]])
                q2 = fc % 2
                P.op("scalar", lambda e, q2=q2: e.copy(out=c_sb[q2], in_=ps[:, 3, :]), reads=[bank[3]], writes=[c_sbB[q2]])
                P.op("vector", lambda e, fc=fc, q2=q2: e.tensor_tensor(out=zc[:, fc, 2:514], in0=c_sb[q2], in1=ps[:, 4, :],
                                                                       op=ALU.mult),
                     reads=[c_sbB[q2], bank[4]], writes=[zcB[fc]])
                P.op("scalar", lambda e, q2=q2: e.copy(out=b_sb[q2], in_=ps[:, 2, :]), reads=[bank[2]], writes=[b_sbB[q2]])
                P.op("scalar", lambda e, fc=fc, q2=q2: e.activation(out=acc[q2], in_=zc[:, fc, 2:514], func=AF.Copy,
                                                                    scale=wconvT[:, fc, 2:3]),
                     reads=[zcB[fc], wconvB], writes=[accB[q2]])

                P.op("vector", lambda e, fc=fc, q2=q2: e.scalar_tensor_tensor(
                    out=acc[q2], in0=zc[:, fc, 1:513], scalar=wconvT[:, fc, 1:2], in1=acc[q2], op0=ALU.mult, op1=ALU.add),
                    reads=[zcB[fc], wconvB], writes=[accB[q2]])
                P.op("vector", lambda e, fc=fc, q2=q2: e.scalar_tensor_tensor(
                    out=acc[q2], in0=zc[:, fc, 0:512], scalar=wconvT[:, fc, 0:1], in1=acc[q2], op0=ALU.mult, op1=ALU.add),
                    reads=[zcB[fc], wconvB], writes=[accB[q2]])
                P.op("gpsimd", lambda e, fc=fc: e.tensor_copy(out=zc[:, fc, 0:2], in_=zc[:, fc, 512:514]),
                     writes=[zcB[fc]])
                P.op("gpsimd", lambda e, fc=fc, q2=q2: e.tensor_tensor(out=yT[:, fc, :], in0=acc[q2], in1=b_sb[q2], op=ALU.mult),
                     reads=[accB[q2], b_sbB[q2]], writes=[yTcB[fc]])
            for jl in range(4):
                j = 4 * blk + jl
                cols = slice(128 * jl, 128 * (jl + 1))

                def mm_uv(e, cols=cols):
                    for bi, cg in ((5, 3), (6, 4)):
                        for k in range(KD):
                            i = e.matmul(ps[:, bi, :], lhsT=hT[:, k, cols], rhs=win_bf[cg][:, k, :],
                                         start=(k == 0), stop=(k == KD - 1))
                    return i
                P.op("tensor", mm_uv, reads=[hTB[jl], winB[3], winB[4]], writes=[bank[5], bank[6]])
                jp = j % 2
                P.op("scalar", lambda e, jp=jp: e.activation(out=gu[jp], in_=ps[:, 5, :], func=AF.Gelu_apprx_tanh),
                     reads=[bank[5]], writes=[guB[jp]])
                P.op("scalar", lambda e, j=j, jp=jp: e.activation(out=gv[jp], in_=ps[:, 6, :], func=AF.Gelu_apprx_tanh,
                                                                  accum_out=ss[:, 2, j:j + 1]),
                     reads=[bank[6]], writes=[gvB[jp], ssb(2, j)])

                P.op("gpsimd", lambda e, j=j: e.tensor_scalar(out=ss[:, 3, j:j + 1], in0=ss[:, 2, j:j + 1], scalar1=-1.0 / 512,
                                                              scalar2=0.0, op0=ALU.mult, op1=ALU.add),
                     reads=[ssb(2, j)], writes=[ssb(3, j)])
                P.op("gpsimd", lambda e, j=j: e.tensor_scalar(out=ss[:, 6, j:j + 1], in0=ss[:, 2, j:j + 1], scalar1=1.0 / 512,
                                                              scalar2=0.0, op0=ALU.mult, op1=ALU.add),
                     reads=[ssb(2, j)], writes=[ssb(6, 100 + j)])
                P.op("scalar", lambda e, j=j, jp=jp: e.activation(out=tmpv, in_=gv[jp], func=AF.Square,
                                                                  bias=ss[:, 3, j:j + 1], accum_out=ss[:, 4, j:j + 1]),
                     reads=[gvB[jp], ssb(3, j)], writes=[tmpvB, ssb(4, j)])
                rstd2(ss[:, 4, j:j + 1], ss[:, 5, j:j + 1], 512, ssb(4, j), ssb(5, j))
                P.op("vector", lambda e, j=j, jp=jp: e.scalar_tensor_tensor(
                    out=tmpv, in0=gv[jp], scalar=ss[:, 6, j:j + 1], in1=gsguB_t, op0=ALU.subtract, op1=ALU.mult),
                    reads=[gvB[jp], ssb(6, 100 + j), gsguB], writes=[tmpvB])
                P.op("scalar", lambda e, j=j: e.activation(out=vn, in_=tmpv, func=AF.Copy, scale=ss[:, 5, j:j + 1]),
                     reads=[tmpvB, ssb(5, j)], writes=[vnB])

                def mm_spatial(e):
                    e.matmul(ps[:, 7, :], lhsT=bhi, rhs=R_bf, start=True, stop=False)
                    e.matmul(ps[:, 7, :], lhsT=blo, rhs=R_bf, start=False, stop=False)
                    for h in range(8):
                        i = e.matmul(ps[:, 7, 64 * h:64 * (h + 1)], lhsT=wsT[:, h, :], rhs=vn[:, 64 * h:64 * (h + 1)],
                                     start=False, stop=(h == 7))
                    return i
                P.op("tensor", mm_spatial, reads=[vnB, wsTB, bsp2B, bsp4B, RB], writes=[bank[7]])
                P.op("vector", lambda e, jp=jp: e.tensor_tensor(out=ysg, in0=gu[jp], in1=ps[:, 7, :], op=ALU.mult),
                     reads=[guB[jp], bank[7]], writes=[ysgB])
                P.op("tensor", tr_generic(ysg, 1, 4), reads=[ysgB, identB], writes=[bank[1]])
                P.op("scalar", lambda e, cols=cols: e.copy(out=yT[:, 4:8, cols], in_=bank_bf(1, 4)),
                     reads=[bank[1]], writes=[yTsB[jl]])
            for jl in range(4):
                j = 4 * blk + jl
                cols = slice(128 * jl, 128 * (jl + 1))

                def mm_out(e, cols=cols):
                    for half in range(2):
                        for k in range(KD):
                            i = e.matmul(ps[:, 2 + half, :], lhsT=yT[:, k, cols], rhs=wout_bf[:, k, 512 * half:512 * (half + 1)],
                                         start=(k == 0), stop=(k == KD - 1))
                    return i
                P.op("tensor", mm_out, reads=yTcB + [yTsB[jl], woutB], writes=[bank[2], bank[3]])
                P.op("vector", lambda e, j=j: e.tensor_tensor(out=xres[:, j, :], in0=xres[:, j, :],
                                                              in1=ps[:, 2:4, :].rearrange("p a b -> p (a b)"), op=ALU.add),
                     reads=[bank[2], bank[3]], writes=[xresB[j]])
                if DEBUG:
                    P.op("sync", lambda e, j=j: e.dma_start(out=dbg_x2[128 * j:128 * (j + 1), :], in_=xres[:, j, :]),
                         reads=[xresB[j]], dma=s_dbg)
                    if jl == 0:
                        P.op("sync", lambda e, blk=blk: e.dma_start(out=dbg_yT[blk], in_=yT),
                             reads=yTcB + yTsB, dma=s_dbg)
                P.op("scalar", lambda e, j=j: e.activation(out=h2t[j % 2], in_=xres[:, j, :], func=AF.Square,
                                                           accum_out=ss[:, 0, j:j + 1]),
                     reads=[xresB[j]], writes=[h2tB[j % 2], ssb(0, j)])
                rstd2(ss[:, 0, j:j + 1], ss[:, 1, j:j + 1], D, ssb(0, j), ssb(1, j))
                P.op("vector", lambda e, j=j: e.scalar_tensor_tensor(
                    out=h2t[j % 2], in0=xres[:, j, :], scalar=ss[:, 1, j:j + 1], in1=gB_[:],
                    op0=ALU.mult, op1=ALU.mult), reads=[xresB[j], ssb(1, j), gBB], writes=[h2tB[j % 2]])
                P.op("sync", lambda e, j=j: e.dma_start(out=h2buf_d[128 * j:128 * (j + 1), :], in_=h2t[j % 2]),
                     reads=[h2tB[j % 2]], writes=[h2bufB[j]], dma=s_h2[j % 2])
                P.op("tensor", tr_generic(h2t[j % 2], 1, 8), reads=[h2tB[j % 2], identB], writes=[bank[1]])
                P.op("vector", lambda e, j=j: e.tensor_copy(out=h2T[j % 2], in_=bank_bf(1, 8)),
                     reads=[bank[1]], writes=[h2TB[j % 2]])

                def mm_router(e, j=j):
                    for k in range(KD):
                        i = e.matmul(ps[:, 4, 0:36], lhsT=h2T[j % 2][:, k, :], rhs=wr_bf[:, k, :],
                                     start=(k == 0), stop=(k == KD - 1))
                    return i
                P.op("tensor", mm_router, reads=[h2TB[j % 2], wrB], writes=[bank[4]])
                P.op("vector", lambda e, j=j: e.tensor_tensor(out=L[:, j, :], in0=ps[:, 4, 0:36], in1=biasB_t, op=ALU.add),
                     reads=[bank[4], biasB], pwrites=[LB])

        P.barrier()
        apos[0] = 0
        NH = 9
        h2s = [carve([128, D], BF16) for _ in range(3)]
        h2sB = [Buf("h2s%d" % i) for i in range(NH)]
        NW = 3
        wg_bf, wu_bf, wd_bf = [], [], []
        for w_ in range(NW):
            if w_ == NW - 1:
                route_start = apos[0]
            wg_bf.append(carve([128, KD, FE], BF16))
            wu_bf.append(carve([128, KD, FE], BF16))
            wd_bf.append(carve([128, 4, D], BF16))
        wslotB = [Buf("wslot%d" % i) for i in range(NW)]
        xe = [carve([128, NS, D], BF16) for _ in range(2)]
        xeB = [[Buf("xe%d_%d" % (i, s)) for s in range(NS)] for i in range(2)]
        xeT = [carve([128, KD, CAP], BF16) for _ in range(2)]
        xeTB = [[Buf("xeT%d_%d" % (i, s)) for s in range(NS)] for i in range(2)]
        hid = [carve([128, 4, CAP], BF16) for _ in range(2)]
        hidB = [[Buf("hid%d_%d" % (i, f)) for f in range(4)] for i in range(2)]
        sg = [carve([128, CAP], F32) for _ in range(2)]
        sgB = [Buf("sg%d" % i) for i in range(2)]
        ye = [carve([128, D], F32) for _ in range(3)]
        yeB = [Buf("ye%d" % i) for i in range(3)]
        for i in range(3):
            h2s.append(ye[i][:, 0:512].bitcast(BF16))
            h2s.append(ye[i][:, 512:1024].bitcast(BF16))
        moe_end = apos[0]
        assert moe_end <= ARENA, "MoE arena overflow %d" % moe_end
        apos[0] = route_start

        def rt(shape=(128, NT, 32), dt=F32):
            return carve(list(shape), dt)
        gmax = rt((128, NT))
        gsh = rt((128, NT, 4))
        gsum = rt((128, NT))
        gw = rt((128, NT))
        gmask = rt((128, NT, 4))
        em = rt()
        m1 = rt((128, NT))
        mask1 = rt()
        em2 = rt()
        m2 = rt((128, NT))
        mask2 = rt()
        dd = rt((128, NT))
        sel = rt((128, NT, 32), BF16)
        off = rt()
        pos = rt()
        ovf = rt()
        val = rt()
        slot = rt()
        t1 = rt()
        idxf = rt((128, NT))
        tot = rt((128, NE))
        scanA = rt((128, 16 + NE))
        scanB = rt((128, 16 + NE))
        eoff = rt((128, NE))
        assert apos[0] <= L_start, "routing tiles overflow %d" % apos[0]

        gl = L[:, :, 0:4]
        el = L[:, :, 4:36]

        def bc(a, n):
            return a.unsqueeze(2).to_broadcast([128, NT, n])

        def rop(eng, fn, reads=()):
            P.op(eng, fn, reads=list(reads), writes=[routeB], strict=True)

        rop("vector", lambda e: e.tensor_reduce(out=gmax, in_=gl, axis=AX.X, op=ALU.max), reads=[LB])
        rop("vector", lambda e: e.tensor_tensor(out=gsh, in0=gl, in1=bc(gmax, 4), op=ALU.subtract))
        rop("vector", lambda e: e.tensor_tensor(out=gmask, in0=gl, in1=bc(gmax, 4), op=ALU.is_equal))
        rop("vector", lambda e: e.tensor_scalar(out=gmask, in0=gmask, scalar1=BIG, scalar2=-BIG, op0=ALU.mult, op1=ALU.add))
        rop("vector", lambda e: e.tensor_tensor(out=em.rearrange("p j (g x) -> p j g x", g=4),
                                                in0=el.rearrange("p j (g x) -> p j g x", g=4),
                                                in1=gmask.unsqueeze(3).to_broadcast([128, NT, 4, 8]), op=ALU.add))
        rop("vector", lambda e: e.tensor_reduce(out=m1, in_=em, axis=AX.X, op=ALU.max))
        rop("vector", lambda e: e.tensor_tensor(out=mask1, in0=em, in1=bc(m1, 32), op=ALU.is_equal))
        rop("vector", lambda e: e.scalar_tensor_tensor(out=em2, in0=mask1, scalar=-BIG, in1=em, op0=ALU.mult, op1=ALU.add))
        rop("vector", lambda e: e.tensor_reduce(out=m2, in_=em2, axis=AX.X, op=ALU.max))
        rop("vector", lambda e: e.tensor_tensor(out=mask2, in0=em2, in1=bc(m2, 32), op=ALU.is_equal))
        rop("vector", lambda e: e.tensor_tensor(out=dd, in0=m2, in1=m1, op=ALU.subtract))
        rop("vector", lambda e: e.tensor_tensor(out=sel, in0=mask1, in1=mask2, op=ALU.add))

        rop("scalar", lambda e: e.activation(out=gsh, in_=gsh, func=AF.Exp))
        rop("scalar", lambda e: e.activation(out=dd, in_=dd, func=AF.Exp))
        def mm_pos(e):
            sel2 = sel.rearrange("p j x -> p (j x)")
            e.matmul(ps[:, 2, :], lhsT=ltri[:], rhs=sel2, start=True, stop=True)
            return e.matmul(ps[:, 3, :], lhsT=ones_bf[:], rhs=sel2, start=True, stop=True)
        P.op("tensor", mm_pos, reads=[routeB, constB, identB], writes=[bank[2], bank[3]])

        rop("vector", lambda e: e.tensor_reduce(out=gsum, in_=gsh, axis=AX.X, op=ALU.add))
        rop("vector", lambda e: e.reciprocal(out=gw, in_=gsum))
        rop("vector", lambda e: e.tensor_scalar(out=gsum, in0=dd, scalar1=1.0, scalar2=None, op0=ALU.add))
        rop("vector", lambda e: e.reciprocal(out=w1g[:], in_=gsum))
        rop("vector", lambda e: e.tensor_tensor(out=w1g[:], in0=w1g[:], in1=gw, op=ALU.mult))
        rop("vector", lambda e: e.tensor_tensor(out=w2g[:], in0=dd, in1=w1g[:], op=ALU.mult))
        cnt_sb = t1
        rop("vector", lambda e: e.tensor_copy(out=cnt_sb, in_=ps[:, 3, :].rearrange("p (j x) -> p j x", j=NT)), reads=[bank[3]])
        rop("vector", lambda e: e.memset(off[:, 0, :], 0.0))
        for j in range(1, NT):
            rop("vector", lambda e, j=j: e.tensor_tensor(out=off[:, j, :], in0=off[:, j - 1, :], in1=cnt_sb[:, j - 1, :], op=ALU.add))
        rop("vector", lambda e: e.tensor_tensor(out=tot, in0=off[:, NT - 1, :], in1=cnt_sb[:, NT - 1, :], op=ALU.add))
        rop("vector", lambda e: e.tensor_tensor(out=pos, in0=ps[:, 2, :].rearrange("p (j x) -> p j x", j=NT), in1=off, op=ALU.add),
            reads=[bank[2]])
        rop("vector", lambda e: e.tensor_scalar(out=ovf, in0=pos, scalar1=float(CAP), scalar2=1.0e6, op0=ALU.is_ge, op1=ALU.mult))
        rop("vector", lambda e: e.tensor_scalar(out=val, in0=pos, scalar1=float(CAP), scalar2=None, op0=ALU.is_lt))
        rop("vector", lambda e: e.memset(scanA, 0.0))
        rop("vector", lambda e: e.memset(scanB, 0.0))
        rop("vector", lambda e: e.tensor_scalar(out=tot, in0=tot, scalar1=float(CAP), scalar2=None, op0=ALU.min))
        rop("vector", lambda e: e.tensor_copy(out=scanA[:, 16:16 + NE], in_=tot))
        src_, dst_ = scanA, scanB
        for sh in (1, 2, 4, 8, 16):
            rop("vector", lambda e, src_=src_, dst_=dst_, sh=sh: e.tensor_tensor(
                out=dst_[:, 16:16 + NE], in0=src_[:, 16:16 + NE], in1=src_[:, 16 - sh:16 + NE - sh], op=ALU.add))
            src_, dst_ = dst_, src_
        rop("vector", lambda e, src_=src_: e.tensor_tensor(out=eoff, in0=src_[:, 16:16 + NE], in1=tot, op=ALU.subtract))
        rop("vector", lambda e: e.tensor_tensor(out=slot, in0=pos, in1=eoff.unsqueeze(1).to_broadcast([128, NT, NE]), op=ALU.add))
        rop("vector", lambda e: e.tensor_tensor(out=slot, in0=slot, in1=ovf, op=ALU.add))
        for (mk, idx, wk) in ((mask1, idx1, w1g), (mask2, idx2, w2g)):
            rop("vector", lambda e, mk=mk: e.tensor_tensor(out=t1, in0=mk, in1=slot, op=ALU.mult))
            rop("vector", lambda e: e.tensor_reduce(out=idxf, in_=t1, axis=AX.X, op=ALU.add))
            rop("vector", lambda e, idx=idx: e.tensor_copy(out=idx[:], in_=idxf))
            rop("vector", lambda e, mk=mk: e.tensor_tensor(out=t1, in0=mk, in1=val, op=ALU.mult))
            rop("vector", lambda e: e.tensor_reduce(out=idxf, in_=t1, axis=AX.X, op=ALU.add))
            rop("vector", lambda e, wk=wk: e.tensor_tensor(out=wk[:], in0=wk[:], in1=idxf, op=ALU.mult))

        srank = ovf.rearrange("p j x -> p (j x)")[:, 0:NE * NS].rearrange("p (e s) -> p e s", e=NE)
        sbase = val.rearrange("p j x -> p (j x)")[:, 0:NE * NS].rearrange("p (e s) -> p e s", e=NE)
        P.op("gpsimd", lambda e: e.iota(srank, pattern=[[0, NE], [128, NS]], base=0, channel_multiplier=1,
                                        allow_small_or_imprecise_dtypes=True), reads=[routeB], writes=[sidxB], strict=True)
        P.op("gpsimd", lambda e: e.iota(sbase, pattern=[[0, NE], [128, NS]], base=0, channel_multiplier=1,
                                        allow_small_or_imprecise_dtypes=True), reads=[routeB], writes=[sidxB], strict=True)
        P.op("vector", lambda e: e.tensor_tensor(out=sbase, in0=sbase, in1=eoff.unsqueeze(2).to_broadcast([128, NE, NS]), op=ALU.add),
             reads=[routeB], writes=[sidxB], strict=True)
        P.op("vector", lambda e: e.tensor_tensor(out=srank, in0=srank, in1=tot.unsqueeze(2).to_broadcast([128, NE, NS]),
                                                 op=ALU.is_ge), writes=[sidxB], strict=True)
        P.op("vector", lambda e: e.scalar_tensor_tensor(out=sbase, in0=srank, scalar=1.0e6, in1=sbase, op0=ALU.mult, op1=ALU.add),
             writes=[sidxB], strict=True)
        P.op("vector", lambda e: e.tensor_copy(out=sidx_i[:], in_=sbase), writes=[sidxB], strict=True)
        if DEBUG:
            for nm, apx in (("gmax", gmax), ("gsh", gsh), ("gmask", gmask), ("em", em), ("m1", m1), ("mask1", mask1),
                            ("em2", em2), ("m2", m2), ("mask2", mask2), ("dd", dd), ("off", off), ("pos", pos),
                            ("slot", slot), ("gw", gw), ("val", val)):
                dt_ = nc.dram_tensor("dbg_" + nm, list(apx.shape), F32, kind="ExternalOutput").ap()
                P.op("sync", lambda e, dt_=dt_, apx=apx: e.dma_start(out=dt_, in_=apx), reads=[routeB], dma=s_dbg)
            P.op("sync", lambda e: e.dma_start(out=dbg_L, in_=L), reads=[LB], dma=s_dbg)
            P.op("sync", lambda e: e.dma_start(out=dbg_i1, in_=idx1[:]), reads=[routeB], dma=s_dbg)
            P.op("sync", lambda e: e.dma_start(out=dbg_i2, in_=idx2[:]), reads=[routeB], dma=s_dbg)
            P.op("sync", lambda e: e.dma_start(out=dbg_w1, in_=w1g[:]), reads=[routeB], dma=s_dbg)
            P.op("sync", lambda e: e.dma_start(out=dbg_w2, in_=w2g[:]), reads=[routeB], dma=s_dbg)
        s_h2s = [P.dsem("s_h2s%d" % i) for i in range(NH)]
        s_scat = [P.dsem("s_scat%d" % i) for i in range(NH)]
        for j in range(NT):
            P.op("sync", lambda e, j=j: e.dma_start(out=h2s[j % NH], in_=h2buf_d[128 * j:128 * (j + 1), :]),
                 reads=[h2bufB[j]], writes=[h2sB[j % NH]], dma=s_h2s[j % NH])
            for idx in (idx1, idx2):
                P.op("gpsimd", lambda e, j=j, idx=idx: e.indirect_dma_start(
                    out=xbuf_d, out_offset=bass.IndirectOffsetOnAxis(ap=idx[:, j:j + 1], axis=0),
                    in_=h2s[j % NH], in_offset=None, bounds_check=bc_reg(e), oob_is_err=False),
                    reads=[h2sB[j % NH], routeB], pwrites=[xbufB], dma=s_scat[j % NH])

        s_w = [P.dsem("s_w%d" % i) for i in range(NW)]
        s_xe = [[P.dsem("s_xe%d_%d" % (i, s)) for s in range(NS)] for i in range(2)]
        s_ye = [P.dsem("s_ye%d" % i) for i in range(3)]
        for ex in range(NE):
            w = ex % NW
            s2 = ex % 2
            wdep = [routeB, sidxB] if w == NW - 1 else []
            P.op("gpsimd", lambda e, ex=ex, w=w: e.dma_start(
                out=wg_bf[w], in_=wg_d[ex].rearrange("(k p) f -> p k f", p=128)), reads=wdep, pwrites=[wslotB[w]], dma=s_w[w])
            P.op("gpsimd", lambda e, ex=ex, w=w: e.dma_start(
                out=wu_bf[w], in_=wu_d[ex].rearrange("(k p) f -> p k f", p=128)), reads=wdep, pwrites=[wslotB[w]], dma=s_w[w])
            P.op("gpsimd", lambda e, ex=ex, w=w: e.dma_start(
                out=wd_bf[w], in_=wd_d[ex].rearrange("(k p) f -> p k f", p=128)), reads=wdep, pwrites=[wslotB[w]], dma=s_w[w])
            for st in range(NS):
                P.op("gpsimd", lambda e, ex=ex, s2=s2, st=st: e.indirect_dma_start(
                    out=xe[s2][:, st, :], out_offset=None, in_=xbuf_d,
                    in_offset=bass.IndirectOffsetOnAxis(ap=sidx_i[:, ex, st:st + 1], axis=0),
                    bounds_check=bc_reg(e), oob_is_err=False),
                    reads=[xbufB, sidxB, routeB], writes=[xeB[s2][st]], dma=s_xe[s2][st], bscale=0.34)
            for st in range(NS):
                tb = st % 2
                P.op("tensor", tr_generic(xe[s2][:, st, :], tb, 8), reads=[xeB[s2][st], identB], writes=[bank[tb]])
                if st % 2 == 0:
                    P.op("scalar", lambda e, s2=s2, st=st, tb=tb: e.copy(out=xeT[s2][:, :, 128 * st:128 * (st + 1)], in_=bank_bf(tb, 8)),
                         reads=[bank[tb]], writes=[xeTB[s2][st]])
                else:
                    P.op("vector", lambda e, s2=s2, st=st, tb=tb: e.tensor_copy(out=xeT[s2][:, :, 128 * st:128 * (st + 1)], in_=bank_bf(tb, 8)),
                         reads=[bank[tb]], writes=[xeTB[s2][st]])
            for fc in range(4):
                bg = 2 + 2 * (fc % 2)
                bu = bg + 1

                def mm_gu(e, fc=fc, bg=bg, bu=bu, w=w, s2=s2):
                    for bi, wt in ((bg, wg_bf[w]), (bu, wu_bf[w])):
                        for k in range(KD):
                            i = e.matmul(ps[:, bi, 0:CAP], lhsT=wt[:, k, 128 * fc:128 * (fc + 1)],
                                         rhs=xeT[s2][:, k, :], start=(k == 0), stop=(k == KD - 1))
                    return i
                P.op("tensor", mm_gu, reads=[wslotB[w]] + xeTB[s2], writes=[bank[bg], bank[bu]])
                P.op("scalar", lambda e, bg=bg, fc=fc: e.activation(out=sg[fc % 2], in_=ps[:, bg, 0:CAP], func=AF.Silu),
                     reads=[bank[bg]], writes=[sgB[fc % 2]])
                P.op("vector", lambda e, bu=bu, fc=fc, s2=s2: e.tensor_tensor(
                    out=hid[s2][:, fc, :], in0=sg[fc % 2], in1=ps[:, bu, 0:CAP], op=ALU.mult),
                    reads=[sgB[fc % 2], bank[bu]], writes=[hidB[s2][fc]])
            for st in range(NS):
                yi = (ex * NS + st) % 3

                def mm_down(e, st=st, w=w, s2=s2):
                    for dh in range(2):
                        for fc in range(4):
                            i = e.matmul(ps[:, 6 + dh, :], lhsT=hid[s2][:, fc, 128 * st:128 * (st + 1)],
                                         rhs=wd_bf[w][:, fc, 512 * dh:512 * (dh + 1)], start=(fc == 0), stop=(fc == 3))
                    return i
                P.op("tensor", mm_down, reads=hidB[s2] + [wslotB[w]], writes=[bank[6], bank[7]])
                src = ps[:, 6:8, :].rearrange("p a b -> p (a b)")
                if (ex * NS + st) % 2 == 0:
                    P.op("scalar", lambda e, yi=yi, src=src: e.copy(out=ye[yi], in_=src),
                         reads=[bank[6], bank[7]], writes=[yeB[yi]])
                else:
                    P.op("vector", lambda e, yi=yi, src=src: e.tensor_copy(out=ye[yi], in_=src),
                         reads=[bank[6], bank[7]], writes=[yeB[yi]])
                P.op("gpsimd", lambda e, ex=ex, st=st, yi=yi: e.indirect_dma_start(
                    out=ybuf_d, out_offset=bass.IndirectOffsetOnAxis(ap=sidx_i[:, ex, st:st + 1], axis=0),
                    in_=ye[yi], in_offset=None, bounds_check=bc_reg(e), oob_is_err=False),
                    reads=[yeB[yi], sidxB], pwrites=[ybufB], dma=s_ye[yi], bscale=0.34)

        P.barrier()
        apos[0] = 0
        NY = 4
        NP3 = 3
        y1 = [carve([128, D], F32) for _ in range(NY)]
        y2 = [carve([128, D], F32) for _ in range(NY)]
        y1B = [Buf("y1_%d" % i) for i in range(NY)]
        y2B = [Buf("y2_%d" % i) for i in range(NY)]
        h3t = [carve([128, D], BF16) for _ in range(NP3)]
        h3tB = [Buf("h3t%d" % i) for i in range(NP3)]
        h3T = [carve([128, KD, 128], BF16) for _ in range(NP3)]
        h3TB = [Buf("h3T%d" % i) for i in range(NP3)]
        pT = [carve([128, 2, 128], BF16) for _ in range(NP3)]
        pTB = [Buf("pT%d" % i) for i in range(NP3)]
        sgm = [carve([128, D], F32) for _ in range(NP3)]
        sgmB = [Buf("sgm%d" % i) for i in range(NP3)]
        tmul = [carve([128, D], F32) for _ in range(NP3)]
        tmulB = [Buf("tmul%d" % i) for i in range(NP3)]
        ot = [carve([128, D], F32) for _ in range(NP3)]
        otB = [Buf("ot%d" % i) for i in range(NP3)]
        assert apos[0] <= ARENA
        ple_w0 = apos[0]
        wpg_bf = carve([128, KD, D], BF16)
        wpp_bf = carve([128, 2, D], BF16)
        p_bf = carve([128, NT, 256], BF16)
        assert apos[0] <= ARENA, "PLE weight prefetch region overflows the arena: %d" % apos[0]
        wpB, pbB = Buf("wple"), Buf("pbf")
        s_p3 = [P.dsem("s_p3_%d" % i) for i in range(4)]
        def ld_gple(e):
            with nc.allow_non_contiguous_dma(reason="gain vector as per-partition columns"):
                for k in range(KD):
                    i = e.dma_start(out=gplec[:, k:k + 1], in_=gple_d[0:1, 128 * k:128 * (k + 1)].rearrange("o p -> p o"))
                    if k < KD - 1:
                        i.then_inc(s_p3[0].h, 16)
            return i
        s_p3[0].n += 16 * (KD - 1)
        P.op("sync", ld_gple, writes=[gplecB], dma=s_p3[0])
        P.op("sync", lambda e: e.dma_start(out=gB_[:], in_=gfin_d.partition_broadcast(128)), writes=[gBB], dma=s_p3[1])
        P.op("gpsimd", lambda e: e.dma_start(out=wpg_bf, in_=wpg_d.rearrange("(k p) f -> p k f", p=128)),
             pwrites=[wpB], dma=s_p3[2])
        P.op("gpsimd", lambda e: e.dma_start(out=wpp_bf, in_=wpp_d.rearrange("(k p) f -> p k f", p=128)),
             pwrites=[wpB], dma=s_p3[2])
        P.op("gpsimd", lambda e: e.dma_start(out=p_bf, in_=p_d.rearrange("(j p) f -> p j f", p=128)),
             writes=[pbB], dma=s_p3[3])
        for k in range(KD):
            P.op("scalar", lambda e, k=k: e.activation(out=wpg_bf[:, k, :], in_=wpg_bf[:, k, :], func=AF.Copy, scale=gplec[:, k:k + 1]),
                 reads=[gplecB], writes=[wpB])


        s_y1 = [P.dsem("s_y1_%d" % i) for i in range(NY)]
        s_y2 = [P.dsem("s_y2_%d" % i) for i in range(NY)]
        s_out = [P.dsem("s_out%d" % i) for i in range(NP3)]
        out_ops = {}
        for j in range(NT):
            s2 = j % NP3
            b2 = j % 2
            sy = j % NY
            P.op("gpsimd", lambda e, j=j, sy=sy: e.indirect_dma_start(
                out=y1[sy], out_offset=None, in_=ybuf_d,
                in_offset=bass.IndirectOffsetOnAxis(ap=idx1[:, j:j + 1], axis=0),
                bounds_check=bc_reg(e), oob_is_err=False), reads=[ybufB, routeB], writes=[y1B[sy]], dma=s_y1[sy])
            P.op("gpsimd", lambda e, j=j, sy=sy: e.indirect_dma_start(
                out=y2[sy], out_offset=None, in_=ybuf_d,
                in_offset=bass.IndirectOffsetOnAxis(ap=idx2[:, j:j + 1], axis=0),
                bounds_check=bc_reg(e), oob_is_err=False), reads=[ybufB, routeB], writes=[y2B[sy]], dma=s_y2[sy])

            P.op("vector", lambda e, j=j, sy=sy: e.scalar_tensor_tensor(
                out=xres[:, j, :], in0=y1[sy], scalar=w1g[:, j:j + 1], in1=xres[:, j, :], op0=ALU.mult, op1=ALU.add),
                reads=[y1B[sy], routeB], writes=[xresB[j]])
            P.op("vector", lambda e, j=j, sy=sy: e.scalar_tensor_tensor(
                out=xres[:, j, :], in0=y2[sy], scalar=w2g[:, j:j + 1], in1=xres[:, j, :], op0=ALU.mult, op1=ALU.add),
                reads=[y2B[sy], routeB], writes=[xresB[j]])
            if DEBUG:
                P.op("sync", lambda e, j=j: e.dma_start(out=dbg_x3[128 * j:128 * (j + 1), :], in_=xres[:, j, :]),
                     reads=[xresB[j]], dma=s_dbg)
            P.op("scalar", lambda e, j=j, s2=s2: e.activation(out=h3t[s2], in_=xres[:, j, :], func=AF.Square,
                                                              accum_out=ss[:, 0, j:j + 1]),
                 reads=[xresB[j]], writes=[h3tB[s2], ssb(0, j)])
            rstd2(ss[:, 0, j:j + 1], ss[:, 1, j:j + 1], D, ssb(0, j), ssb(1, j))
            P.op("scalar", lambda e, j=j, s2=s2: e.activation(out=h3t[s2], in_=xres[:, j, :], func=AF.Copy, scale=ss[:, 1, j:j + 1]),
                 reads=[xresB[j], ssb(1, j)], writes=[h3tB[s2]])
            P.op("tensor", tr_generic(h3t[s2], b2, 8), reads=[h3tB[s2], identB], writes=[bank[b2]])
            P.op("scalar", lambda e, s2=s2, b2=b2: e.copy(out=h3T[s2], in_=bank_bf(b2, 8)), reads=[bank[b2]], writes=[h3TB[s2]])
            P.op("tensor", tr_generic(p_bf[:, j, :], 6 + b2, 2), reads=[pbB, identB], writes=[bank[6 + b2]])
            P.op("vector", lambda e, s2=s2, b2=b2: e.tensor_copy(out=pT[s2], in_=bank_bf(6 + b2, 2)), reads=[bank[6 + b2]], writes=[pTB[s2]])

            def mm_pg(e, s2=s2):
                for half in range(2):
                    for k in range(KD):
                        i = e.matmul(ps[:, 2 + half, :], lhsT=h3T[s2][:, k, :], rhs=wpg_bf[:, k, 512 * half:512 * (half + 1)],
                                     start=(k == 0), stop=(k == KD - 1))
                return i

            def mm_pp(e, s2=s2):
                for half in range(2):
                    for k in range(2):
                        i = e.matmul(ps[:, 4 + half, :], lhsT=pT[s2][:, k, :], rhs=wpp_bf[:, k, 512 * half:512 * (half + 1)],
                                     start=(k == 0), stop=(k == 1))
                return i
            P.op("tensor", mm_pg, reads=[h3TB[s2], wpB], writes=[bank[2], bank[3]])
            P.op("tensor", mm_pp, reads=[pTB[s2], wpB], writes=[bank[4], bank[5]])
            P.op("scalar", lambda e, s2=s2: e.activation(out=sgm[s2], in_=ps[:, 2:4, :].rearrange("p a b -> p (a b)"), func=AF.Sigmoid),
                 reads=[bank[2], bank[3]], writes=[sgmB[s2]])
            P.op("vector", lambda e, s2=s2: e.tensor_tensor(out=tmul[s2], in0=sgm[s2], in1=ps[:, 4:6, :].rearrange("p a b -> p (a b)"),
                                                            op=ALU.mult), reads=[sgmB[s2], bank[4], bank[5]], writes=[tmulB[s2]])
            P.op("gpsimd", lambda e, j=j, s2=s2: e.tensor_tensor(out=xres[:, j, :], in0=xres[:, j, :], in1=tmul[s2], op=ALU.add),
                 reads=[tmulB[s2]], writes=[xresB[j]])
            P.op("scalar", lambda e, j=j, s2=s2: e.activation(out=ot[s2], in_=xres[:, j, :], func=AF.Square,
                                                              accum_out=ss[:, 2, j:j + 1]),
                 reads=[xresB[j]], writes=[otB[s2], ssb(2, j)])
            rstd2(ss[:, 2, j:j + 1], ss[:, 3, j:j + 1], D, ssb(2, j), ssb(3, j))
            P.op("vector", lambda e, j=j, s2=s2: e.scalar_tensor_tensor(
                out=ot[s2], in0=xres[:, j, :], scalar=ss[:, 3, j:j + 1], in1=gB_[:], op0=ALU.mult, op1=ALU.mult),
                reads=[xresB[j], ssb(3, j), gBB], writes=[otB[s2]])
            out_ops[s2] = P.op("sync", lambda e, j=j, s2=s2: e.dma_start(out=out_d[128 * j:128 * (j + 1), :], in_=ot[s2]),
                               reads=[otB[s2]], dma=s_out[s2])
        P.wait_all("sync", list(out_ops.values()))

        block = es.enter_context(nc.Block())
        P.emit(block)
        build_nc.makespan = P.makespan
    return nc


_NC_CACHE = {}


def kernel(x, p, g_mix, w_in, w_conv, g_sgu, w_spatial, b_spatial, w_out, g_ffn,
           w_group, b_group, w_router, b_router, w_gate, w_up, w_down,
           g_ple, w_ple_gate, w_ple_proj, g_final):
    f = lambda a: np.ascontiguousarray(np.asarray(a, dtype=np.float32))
    x = f(x)
    p = f(p)
    shared = {
        "g_mix": f(g_mix).reshape(1, D),
        "w_in": f(w_in)[0],
        "w_conv": f(w_conv)[0],
        "g_sgu": f(g_sgu).reshape(1, 512),
        "w_spatial": f(w_spatial)[0],
        "b_spatial": f(b_spatial)[0],
        "w_out": f(w_out)[0],
        "g_ffn": f(g_ffn).reshape(1, D),
        "w_group": f(w_group)[0],
        "b_group": f(b_group).reshape(1, 4),
        "w_router": f(w_router)[0],
        "b_router": f(b_router).reshape(1, 32),
        "w_gate": f(w_gate)[0].reshape(NE, D, FE),
        "w_up": f(w_up)[0].reshape(NE, D, FE),
        "w_down": f(w_down)[0].reshape(NE, FE, D),
        "g_ple": f(g_ple).reshape(1, D),
        "w_ple_gate": f(w_ple_gate)[0],
        "w_ple_proj": f(w_ple_proj)[0],
        "g_final": f(g_final).reshape(1, D),
    }
    in_maps = []
    for c in range(NCORES):
        b, half = c // 2, c % 2
        t0 = half * TOK
        xc = x[b, t0:t0 + TOK]
        if half == 0:
            xh = np.zeros((2, D), np.float32)
        else:
            xh = x[b, t0 - 2:t0]
        m = dict(shared)
        m["x"] = np.ascontiguousarray(xc)
        m["xh"] = np.ascontiguousarray(xh)
        m["p"] = np.ascontiguousarray(p[0, b, t0:t0 + TOK])
        in_maps.append(m)
    if "nc" not in _NC_CACHE:
        _NC_CACHE["nc"] = build_nc()
    res = run_bass_kernel_spmd(_NC_CACHE["nc"], in_maps, core_ids=list(range(NCORES)))
    out = np.empty((4, 4096, D), np.float32)
    for c in range(NCORES):
        b, half = c // 2, c % 2
        out[b, half * TOK:(half + 1) * TOK] = res.results[c]["out"]
    return out
```

```python
import numpy as np
from contextlib import ExitStack

import concourse.bass as bass
import concourse.mybir as mybir
from concourse.bass_utils import run_bass_kernel_spmd

F32 = mybir.dt.float32
BF16 = mybir.dt.bfloat16
I32 = mybir.dt.int32
AF = mybir.ActivationFunctionType
ALU = mybir.AluOpType
AX = mybir.AxisListType

NCORES = 8
TOK = 2048
NT = 16
D = 1024
KD = 8
NE = 32
CAP = 384
NS = CAP // 128
FE = 512
EPS = 1e-6
BIG = 1.0e30
DEBUG = False
SYNC_ALL = True

ENGS = ("sync", "scalar", "vector", "gpsimd", "tensor")


class Sem:
    def __init__(self, h, step):
        self.h = h
        self.step = step
        self.n = 0
        self.last = None


class Buf:
    __slots__ = ("name", "w", "pw", "r")

    def __init__(self, name):
        self.name = name
        self.w = None
        self.pw = []
        self.r = []


class Op:
    __slots__ = ("eng", "fn", "dma", "target", "signal", "count", "deps", "odeps", "cidx", "pos", "strict",
                 "cost", "nbytes", "nleft", "kids", "okids", "ready", "fin", "iss")


DEF_COST = {"sync": 60.0, "scalar": 300.0, "vector": 250.0, "gpsimd": 500.0, "tensor": 300.0}
DMA_BW = 0.33e3
DMA_LAT = 2000.0


class _Dummy:
    def then_inc(self, *a, **k):
        return self


def _nfree(ap):
    n = 1
    for d in ap.shape[1:]:
        n *= int(d)
    return n


class CostProxy:
    def __init__(self, eng):
        self.eng = eng
        self.cost = 0.0
        self.nbytes = 0

    def to_reg(self, v):
        return None

    def __getattr__(self, name):
        def f(*args, **kw):
            out = kw.get("out", args[0] if args else None)
            eng = self.eng
            if name in ("dma_start", "indirect_dma_start"):
                src = kw.get("in_", args[1] if len(args) > 1 else out)
                n = min(_nfree(out) * int(out.shape[0]), _nfree(src) * int(src.shape[0]))
                self.nbytes += n * (2 if src.dtype == BF16 else 4)
                self.cost += {"sync": 80.0, "scalar": 80.0}.get(eng, 2500.0 if name.startswith("ind") else 1200.0)
            elif eng == "tensor":
                if name == "matmul":
                    rhs = kw.get("rhs", args[2] if len(args) > 2 else None)
                    self.cost += max(_nfree(rhs), 64) / 2.0 + 6.0
                else:
                    self.cost += 70.0
            elif eng == "scalar":
                self.cost += 224.0 + 0.833 * _nfree(out) + (60.0 if kw.get("accum_out") is not None else 0.0)
            elif eng == "vector":
                src = kw.get("in_", kw.get("in0", out))
                self.cost += 70.0 + 1.04 * max(_nfree(out), _nfree(src))
            else:
                self.cost += 300.0 + 2.0 * _nfree(out)
            return _Dummy()
        return f


class Prog:
    def __init__(self, nc, es):
        self.nc = nc
        self.es = es
        self.ops = []
        self.q = {e: [] for e in ENGS}
        self.esem = {e: Sem(es.enter_context(nc.semaphore("eng_" + e)), 1)
                     for e in ("scalar", "vector", "gpsimd", "tensor")}
        self.fence = {e: None for e in ENGS}
        self.since_fence = []

    def dsem(self, name):
        return Sem(self.es.enter_context(self.nc.semaphore(name)), 16)

    def _add(self, eng, fn, deps, dma, strict=False, cost=None, nbytes=0):
        o = Op()
        o.eng = eng
        o.fn = fn
        o.dma = dma
        o.signal = False
        o.count = None
        o.target = None
        o.strict = strict
        if cost is None and fn is not None:
            px = CostProxy(eng)
            fn(px)
            cost, nbytes = px.cost, px.nbytes
        o.cost = DEF_COST[eng] if cost is None else float(cost)
        o.nbytes = nbytes
        deps = list(deps)
        o.odeps = []
        if dma is not None:
            dma.n += 16
            o.target = dma.n
            if dma.last is not None:
                assert dma.last.eng == eng
                o.odeps.append(dma.last)
            dma.last = o
        if self.fence[eng] is not None:
            deps.append(self.fence[eng])
        seen = set()
        o.deps = []
        for d in deps:
            if id(d) not in seen:
                seen.add(id(d))
                o.deps.append(d)
        o.cidx = len(self.ops)
        self.ops.append(o)
        self.since_fence.append(o)
        return o

    def op(self, eng, fn, reads=(), writes=(), pwrites=(), dma=None, strict=False, cost=None, nbytes=0, bscale=1.0):
        deps = []
        for b in reads:
            if b.w is not None:
                deps.append(b.w)
            deps.extend(b.pw)
        for b in tuple(writes) + tuple(pwrites):
            if b.w is not None:
                deps.append(b.w)
            deps.extend(b.r)
        for b in writes:
            deps.extend(b.pw)
        o = self._add(eng, fn, deps, dma, strict, cost, nbytes)
        o.nbytes = o.nbytes * bscale
        for b in writes:
            b.w = o
            b.pw = []
            b.r = []
        for b in pwrites:
            b.pw.append(o)
        for b in reads:
            if b not in writes and b not in pwrites:
                b.r.append(o)
        return o

    def barrier(self):
        prev = list(self.since_fence)
        fs = {}
        for e in ENGS:
            fs[e] = self._add(e, None, prev, None, cost=0.0)
        self.fence = fs
        self.since_fence = []

    def wait_all(self, eng, ops):
        self._add(eng, None, list(ops), None, cost=0.0)

    def schedule(self):
        for o in self.ops:
            o.nleft = len(o.deps) + len(o.odeps)
            o.kids = []
            o.okids = []
            o.ready = 0.0
            o.fin = None
        for o in self.ops:
            for d in o.deps:
                d.kids.append(o)
            for d in o.odeps:
                d.okids.append(o)
        avail = {e: [] for e in ENGS}
        for o in self.ops:
            if o.nleft == 0:
                avail[o.eng].append(o)
        free = {e: 0.0 for e in ENGS}
        dma_free = 0.0
        n_left = len(self.ops)
        LOOK = 24
        while n_left:
            best = None
            for e in ENGS:
                av = avail[e]
                if not av:
                    continue
                av.sort(key=lambda o: o.cidx)
                cand = None
                for o in av[:LOOK]:
                    st = max(free[e], o.ready)
                    if cand is None or st < cand[0] - 1e-9:
                        cand = (st, o)
                if best is None or cand[0] < best[0] - 1e-9 or (abs(cand[0] - best[0]) <= 1e-9 and cand[1].cidx < best[1].cidx):
                    best = cand
            st, o = best
            e = o.eng
            avail[e].remove(o)
            free[e] = st + o.cost
            if o.dma is not None:
                dma_free = max(dma_free, st) + o.nbytes / DMA_BW
                o.fin = max(st + DMA_LAT, dma_free)
            else:
                o.fin = st + o.cost
            o.pos = len(self.q[e])
            self.q[e].append(o)
            n_left -= 1
            for k in o.kids:
                lat = 0.0
                if k.eng == o.eng and o.dma is None and k.dma is None and o.fn is not None:
                    lat = 1500.0 if k.strict else (400.0 if SYNC_ALL else 0.0)
                k.ready = max(k.ready, o.fin + lat)
                k.nleft -= 1
                if k.nleft == 0:
                    avail[k.eng].append(k)
            for k in o.okids:
                k.ready = max(k.ready, st + o.cost)
                k.nleft -= 1
                if k.nleft == 0:
                    avail[k.eng].append(k)
        self.makespan = max(o.fin for o in self.ops)

    def emit(self, block):
        self.schedule()
        for e in ENGS:
            for o in self.q[e]:
                best = {}
                for d in o.deps:
                    if d.fn is None and d.dma is None:
                        assert d.eng == o.eng
                        continue
                    if d.dma is None and o.dma is None and d.eng == o.eng and not (o.strict or SYNC_ALL):
                        assert d.pos < o.pos
                        continue
                    if d.dma is not None:
                        k = ("d", id(d.dma))
                        if k not in best or d.target > best[k].target:
                            best[k] = d
                    else:
                        k = ("e", d.eng)
                        if k not in best or d.pos > best[k].pos:
                            best[k] = d
                for d in o.odeps:
                    assert d.eng == o.eng and d.pos < o.pos
                o.deps = list(best.values())
                for d in o.deps:
                    if d.dma is None:
                        d.signal = True
        for e in ("scalar", "vector", "gpsimd", "tensor"):
            c = 0
            for o in self.q[e]:
                if o.dma is None and o.signal:
                    assert o.fn is not None
                    c += 1
                    o.count = c
        prog = self

        def run(eobj, ename):
            waited = {}
            for o in prog.q[ename]:
                for d in o.deps:
                    if d.dma is not None:
                        sem, val = d.dma.h, d.target
                    else:
                        sem, val = prog.esem[d.eng].h, d.count
                    k = id(sem)
                    if waited.get(k, 0) >= val:
                        continue
                    eobj.wait_ge(sem, val)
                    waited[k] = val
                if o.fn is None:
                    continue
                ins = o.fn(eobj)
                if o.dma is not None:
                    ins.then_inc(o.dma.h, 16)
                elif o.signal:
                    ins.then_inc(prog.esem[ename].h, 1)

        @block.sync
        def _(e):
            run(e, "sync")

        @block.scalar
        def _(e):
            run(e, "scalar")

        @block.vector
        def _(e):
            run(e, "vector")

        @block.gpsimd
        def _(e):
            run(e, "gpsimd")

        @block.tensor
        def _(e):
            run(e, "tensor")


def build_nc():
    nc = bass.Bass("TRN2", target_bir_lowering=False)

    def din(name, shape):
        return nc.dram_tensor(name, list(shape), F32, kind="ExternalInput").ap()

    x_d = din("x", [TOK, D])
    xh_d = din("xh", [2, D])
    p_d = din("p", [TOK, 256])
    gmix_d = din("g_mix", [1, D])
    win_d = din("w_in", [D, 2560])
    wconv_d = din("w_conv", [3, 512])
    gsgu_d = din("g_sgu", [1, 512])
    wsp_d = din("w_spatial", [8, 128, 128])
    bsp_d = din("b_spatial", [8, 128])
    wout_d = din("w_out", [D, D])
    gffn_d = din("g_ffn", [1, D])
    wgrp_d = din("w_group", [D, 4])
    bgrp_d = din("b_group", [1, 4])
    wrt_d = din("w_router", [4, D, 8])
    brt_d = din("b_router", [1, 32])
    wg_d = din("w_gate", [NE, D, FE])
    wu_d = din("w_up", [NE, D, FE])
    wd_d = din("w_down", [NE, FE, D])
    gple_d = din("g_ple", [1, D])
    wpg_d = din("w_ple_gate", [D, D])
    wpp_d = din("w_ple_proj", [256, D])
    gfin_d = din("g_final", [1, D])
    out_d = nc.dram_tensor("out", [TOK, D], F32, kind="ExternalOutput").ap()
    h2buf_d = nc.dram_tensor("h2buf", [TOK, D], BF16, kind="Internal").ap()
    xbuf_d = nc.dram_tensor("xbuf", [2 * TOK, D], BF16, kind="Internal").ap()
    ybuf_d = nc.dram_tensor("ybuf", [2 * TOK, D], F32, kind="Internal").ap()

    if DEBUG:
        dbg_x2 = nc.dram_tensor("dbg_x2", [TOK, D], F32, kind="ExternalOutput").ap()
        dbg_x3 = nc.dram_tensor("dbg_x3", [TOK, D], F32, kind="ExternalOutput").ap()
        dbg_L = nc.dram_tensor("dbg_L", [128, NT, 36], F32, kind="ExternalOutput").ap()
        dbg_i1 = nc.dram_tensor("dbg_i1", [128, NT], I32, kind="ExternalOutput").ap()
        dbg_i2 = nc.dram_tensor("dbg_i2", [128, NT], I32, kind="ExternalOutput").ap()
        dbg_w1 = nc.dram_tensor("dbg_w1", [128, NT], F32, kind="ExternalOutput").ap()
        dbg_w2 = nc.dram_tensor("dbg_w2", [128, NT], F32, kind="ExternalOutput").ap()
        dbg_yT = nc.dram_tensor("dbg_yT", [4, 128, KD, 512], BF16, kind="ExternalOutput").ap()

    with ExitStack() as es:
        def sb(name, shape, dt):
            return es.enter_context(nc.sbuf_tensor(name, list(shape), dt))

        P = Prog(nc, es)
        ps = es.enter_context(nc.psum_tensor("ps", [128, 8, 512], F32))
        bank = [Buf("bank%d" % i) for i in range(8)]

        def bank_bf(b, k):
            return ps[:, b, 0:64 * k].bitcast(BF16).rearrange("p (k t) -> p k t", k=k)

        xres = sb("xres", [128, NT, D], F32)
        xresB = [Buf("xres%d" % j) for j in range(NT)]
        gA = sb("gA", [128, D], F32)
        gB_ = sb("gB", [128, D], F32)
        gAB, gBB = Buf("gA"), Buf("gB")
        ident = sb("ident", [128, 128], BF16)
        ltri = sb("ltri", [128, 128], BF16)
        ones_bf = sb("ones_bf", [128, 128], BF16)
        onesf = sb("onesf", [128, 512], F32)
        mhalf = sb("mhalf", [128, 1], F32)
        gplec = sb("gplec", [128, KD], F32)
        gplecB = Buf("gplec")
        tickB = [[Buf("tick%d_%d" % (b, h)) for h in range(4)] for b in range(4)]
        constB = Buf("const")
        identB = Buf("ident")
        ss = sb("ss", [128, 8, NT], F32)
        _ssB = {}

        def ssb(row, j):
            if (row, j) not in _ssB:
                _ssB[(row, j)] = Buf("ss%d_%d" % (row, j))
            return _ssB[(row, j)]
        idx1 = sb("idx1", [128, NT], I32)
        idx2 = sb("idx2", [128, NT], I32)
        w1g = sb("w1g", [128, NT], F32)
        w2g = sb("w2g", [128, NT], F32)
        routeB = Buf("route")
        sidx_i = sb("sidx_i", [128, NE, NS], I32)
        sidxB = Buf("sidx")
        ARENA = 127 * 1024
        arena = sb("arena", [128, ARENA // 2], BF16)
        apos = [0]

        def carve(shape, dt):
            n = int(np.prod(shape[1:]))
            nb = n * (4 if dt in (F32, I32) else 2)
            nb = (nb + 31) // 32 * 32
            a = apos[0]
            assert a + nb <= ARENA, "arena overflow %d" % (a + nb)
            apos[0] = a + nb
            v = arena[0:shape[0], a // 2:(a + nb) // 2]
            if dt != BF16:
                v = v.bitcast(dt)
            v = v[:, 0:n]
            if len(shape) == 3:
                v = v.rearrange("p (a b) -> p a b", a=shape[1])
            elif len(shape) == 4:
                v = v.rearrange("p (a b c) -> p a b c", a=shape[1], b=shape[2])
            return v

        win_bf = [carve([128, KD, 512], BF16) for _ in range(5)]
        winB = [Buf("win%d" % i) for i in range(5)]
        wout_bf = carve([128, KD, D], BF16)
        woutB = Buf("wout")
        hT = carve([128, KD, 512], BF16)
        hTB = [Buf("hT%d" % i) for i in range(4)]
        htok = [carve([128, D], BF16) for _ in range(2)]
        htokB = [Buf("htok%d" % i) for i in range(2)]
        yT = carve([128, KD, 512], BF16)
        yTcB = [Buf("yTc%d" % i) for i in range(4)]
        yTsB = [Buf("yTs%d" % i) for i in range(4)]
        c_sb = [carve([128, 512], F32) for _ in range(2)]
        b_sb = [carve([128, 512], F32) for _ in range(2)]
        c_sbB = [Buf("c_sb%d" % i) for i in range(2)]
        b_sbB = [Buf("b_sb%d" % i) for i in range(2)]
        zc = carve([128, 4, 514], F32)
        zcB = [Buf("zc%d" % i) for i in range(4)]
        acc = [carve([128, 512], F32) for _ in range(2)]
        accB = [Buf("acc%d" % i) for i in range(2)]
        gu = [carve([128, 512], F32) for _ in range(2)]
        gv = [carve([128, 512], F32) for _ in range(2)]
        tmpv = carve([128, 512], F32)
        guB = [Buf("gu%d" % i) for i in range(2)]
        gvB = [Buf("gv%d" % i) for i in range(2)]
        tmpvB = Buf("tmpv")
        vn = carve([128, 512], BF16)
        ysg = carve([128, 512], BF16)
        vnB, ysgB = Buf("vn"), Buf("ysg")
        a0 = apos[0]
        xh_t = carve([128, D], F32)
        xhB = Buf("xh")
        hTh = carve([128, KD, 128], BF16)
        hThB = Buf("hTh")
        wsp_bf = carve([128, 8, 128], BF16)
        a1 = apos[0]
        apos[0] = a0
        h2t = [carve([128, D], BF16) for _ in range(2)]
        h2tB = [Buf("h2t%d" % i) for i in range(2)]
        h2T = [carve([128, KD, 128], BF16) for _ in range(2)]
        h2TB = [Buf("h2T%d" % i) for i in range(2)]
        apos[0] = max(apos[0], a1)
        wsT = carve([128, 8, 128], BF16)
        wspB, wsTB = Buf("wsp"), Buf("wsT")
        gsguB_t = carve([128, 512], F32)
        gsguB = Buf("gsgu")
        biasB_t = carve([128, 36], F32)
        biasB = Buf("bias")
        wconvT = carve([128, 4, 3], F32)
        wconvB = Buf("wconv")
        R_f = tmpv[0:8, :]
        R_bf = carve([8, 512], BF16)
        RB = Buf("R")
        bsp_f = carve([8, 128], F32)
        bsp_t = carve([8, 128], F32)
        bhi = carve([8, 128], BF16)
        blo = carve([8, 128], BF16)
        bspB = Buf("bsp")
        bsp4B = Buf("bsp4")
        wr_bf = carve([128, KD, 36], BF16)
        wrB = Buf("wr")
        mixer_end = apos[0]
        L_BYTES = NT * 36 * 4
        L_start = ARENA - L_BYTES
        assert mixer_end <= L_start, "mixer arena overflow %d" % mixer_end
        apos[0] = L_start
        L = carve([128, NT, 36], F32)
        LB = Buf("L")

        h2bufB = [Buf("h2buf%d" % j) for j in range(NT)]
        xbufB = Buf("xbuf")
        ybufB = Buf("ybuf")

        s_x = [P.dsem("s_x%d" % b) for b in range(4)]

        def load_x(b, after=()):
            P.op("sync", lambda e, b=b: e.dma_start(
                out=xres[:, 4 * b:4 * b + 4, :],
                in_=x_d[512 * b:512 * (b + 1), :].rearrange("(j p) d -> p j d", p=128)),
                reads=list(after), writes=xresB[4 * b:4 * b + 4], dma=s_x[b])
        load_x(0)
        s_c = [P.dsem("s_c%d" % i) for i in range(12)]
        P.op("sync", lambda e: e.dma_start(out=gA[:], in_=gmix_d.partition_broadcast(128)),
             writes=[gAB], dma=s_c[0])
        P.op("sync", lambda e: e.dma_start(out=gB_[:], in_=gffn_d.partition_broadcast(128)),
             writes=[gBB], dma=s_c[1])
        P.op("sync", lambda e: e.dma_start(out=gsguB_t, in_=gsgu_d.partition_broadcast(128)),
             writes=[gsguB], dma=s_c[2])
        P.op("sync", lambda e: e.dma_start(out=biasB_t[:, 0:4], in_=bgrp_d.partition_broadcast(128)),
             pwrites=[biasB], dma=s_c[3])
        P.op("sync", lambda e: e.dma_start(out=biasB_t[:, 4:36], in_=brt_d.partition_broadcast(128)),
             pwrites=[biasB], dma=s_c[3])

        def ld_wconv(e):
            with nc.allow_non_contiguous_dma(reason="tiny conv taps"):
                n = 0
                for k in range(3):
                    for fc in range(4):
                        i = e.dma_start(out=wconvT[:, fc, k:k + 1],
                                        in_=wconv_d[k:k + 1, 128 * fc:128 * (fc + 1)].rearrange("o p -> p o"))
                        n += 1
                        if n < 12:
                            i.then_inc(s_c[4].h, 16)
                return i
        s_c[4].n += 16 * 11
        P.op("sync", ld_wconv, writes=[wconvB], dma=s_c[4])
        P.op("sync", lambda e: e.dma_start(out=bsp_f, in_=bsp_d), writes=[bspB], dma=s_c[5])

        P.op("gpsimd", lambda e: e.memset(onesf[:], 1.0), writes=[constB])
        P.op("gpsimd", lambda e: e.memset(mhalf[:], -0.5), pwrites=[constB])


        P.op("gpsimd", lambda e: e.memset(ones_bf[:], 1.0), pwrites=[constB])
        P.op("gpsimd", lambda e: e.affine_select(out=ident[:], in_=onesf[:, 0:128], pattern=[[1, 128]],
                                                 compare_op=ALU.is_equal, fill=0.0, base=0, channel_multiplier=-1),
             reads=[constB], pwrites=[identB])
        P.op("gpsimd", lambda e: e.affine_select(out=ltri[:], in_=onesf[:, 0:128], pattern=[[1, 128]],
                                                 compare_op=ALU.is_gt, fill=0.0, base=0, channel_multiplier=-1),
             reads=[constB], pwrites=[identB])

        P.op("gpsimd", lambda e: e.affine_select(out=R_f, in_=onesf[0:8, :], pattern=[[1, 512]],
                                                 compare_op=ALU.is_ge, fill=0.0, base=0, channel_multiplier=-64),
             reads=[constB], writes=[tmpvB])
        P.op("gpsimd", lambda e: e.affine_select(out=R_bf, in_=R_f, pattern=[[-1, 512]],
                                                 compare_op=ALU.is_ge, fill=0.0, base=63, channel_multiplier=64),
             reads=[tmpvB], writes=[RB])

        s_win = [P.dsem("s_win%d" % i) for i in range(5)]
        s_w0 = [P.dsem("s_w0_%d" % i) for i in range(4)]
        P.op("gpsimd", lambda e: e.dma_start(out=wsp_bf, in_=wsp_d.rearrange("h t s -> t h s")),
             writes=[wspB], dma=s_w0[0])
        for cg in (1, 2, 0, 3, 4):
            P.op("gpsimd", lambda e, cg=cg: e.dma_start(
                out=win_bf[cg], in_=win_d[:, 512 * cg:512 * (cg + 1)].rearrange("(k p) f -> p k f", p=128)),
                writes=[winB[cg]], dma=s_win[cg])
        P.op("gpsimd", lambda e: e.dma_start(out=wout_bf, in_=wout_d.rearrange("(k p) f -> p k f", p=128)),
             writes=[woutB], dma=s_w0[1])

        def ld_wr(e):
            with nc.allow_non_contiguous_dma(reason="router weights"):
                e.dma_start(out=wr_bf[:, :, 0:4], in_=wgrp_d.rearrange("(k p) e -> p k e", p=128)).then_inc(s_w0[2].h, 16)
                for g in range(3):
                    e.dma_start(out=wr_bf[:, :, 4 + 8 * g:12 + 8 * g],
                                in_=wrt_d[g].rearrange("(k p) e -> p k e", p=128)).then_inc(s_w0[2].h, 16)
                return e.dma_start(out=wr_bf[:, :, 28:36], in_=wrt_d[3].rearrange("(k p) e -> p k e", p=128))
        s_w0[2].n += 64
        P.op("gpsimd", ld_wr, writes=[wrB], dma=s_w0[2])

        bsp2B, bsp3B = Buf("bsp2"), Buf("bsp3")
        P.op("vector", lambda e: e.tensor_copy(out=bhi, in_=bsp_f), reads=[bspB], writes=[bsp2B])
        P.op("vector", lambda e: e.tensor_tensor(out=bsp_t, in0=bsp_f, in1=bhi, op=ALU.subtract), reads=[bspB, bsp2B], writes=[bsp3B])
        P.op("vector", lambda e: e.tensor_copy(out=blo, in_=bsp_t), reads=[bsp3B], writes=[bsp4B])

        def tr_ws(e):
            for h in range(8):
                i = e.transpose(out=bank_bf(0, 8)[:, h, :], in_=wsp_bf[:, h, :], identity=ident[:])
            return i
        P.op("tensor", tr_ws, reads=[wspB, identB], writes=[bank[0]])
        P.op("vector", lambda e: e.tensor_copy(out=wsT, in_=bank_bf(0, 8)), reads=[bank[0]], writes=[wsTB])
        P.op("gpsimd", lambda e: e.affine_select(out=wsT, in_=wsT, pattern=[[0, 8], [1, 128]],
                                                 compare_op=ALU.is_ge, fill=0.0, base=0, channel_multiplier=-1),
             writes=[wsTB])

        _regs = {}

        def bc_reg(e):
            if isinstance(e, CostProxy):
                return None
            if "bc" not in _regs:
                _regs["bc"] = e.to_reg(2 * TOK - 1)
            return _regs["bc"]

        def rstd2(col_ss, col_out, n, rb, wb, eps=EPS):
            P.op("gpsimd", lambda e: e.tensor_scalar(out=col_out, in0=col_ss, scalar1=1.0 / n, scalar2=eps,
                                                     op0=ALU.mult, op1=ALU.add), reads=[rb], writes=[wb])
            P.op("gpsimd", lambda e: e.tensor_tensor(out=col_out, in0=col_out, in1=mhalf[:], op=ALU.pow),
                 reads=[constB], writes=[wb])

        P.op("gpsimd", lambda e: e.memset(xh_t, 0.0), writes=[xhB])
        s_xh = P.dsem("s_xh")
        P.op("sync", lambda e: e.dma_start(out=xh_t[0:2, :], in_=xh_d), writes=[xhB], dma=s_xh)
        P.op("scalar", lambda e: e.activation(out=htok[0], in_=xh_t, func=AF.Square, accum_out=ss[:, 7, 0:1]),
             reads=[xhB], writes=[htokB[0], ssb(7, 0)])
        rstd2(ss[:, 7, 0:1], ss[:, 7, 1:2], D, ssb(7, 0), ssb(7, 1))
        P.op("vector", lambda e: e.scalar_tensor_tensor(out=htok[0], in0=xh_t, scalar=ss[:, 7, 1:2], in1=gA[:],
                                                        op0=ALU.mult, op1=ALU.mult),
             reads=[xhB, ssb(7, 1), gAB], writes=[htokB[0]])

        def tr_generic(src, nb, k):
            def f(e):
                for kk in range(k):
                    i = e.transpose(out=bank_bf(nb, k)[:, kk, :], in_=src[:, 128 * kk:128 * (kk + 1)], identity=ident[:])
                return i
            return f
        P.op("tensor", tr_generic(htok[0], 0, 8), reads=[htokB[0], identB], writes=[bank[0]])
        P.op("scalar", lambda e: e.copy(out=hTh, in_=bank_bf(0, 8)), reads=[bank[0]], writes=[hThB])

        def mm_halo(e):
            for gi, cg in enumerate((1, 2)):
                for fc in range(4):
                    for k in range(KD):
                        i = e.matmul(ps[:, 1, (gi * 4 + fc) * 2:(gi * 4 + fc) * 2 + 2],
                                     lhsT=win_bf[cg][:, k, 128 * fc:128 * (fc + 1)], rhs=hTh[:, k, 0:2],
                                     start=(k == 0), stop=(k == KD - 1))
            return i
        P.op("tensor", mm_halo, reads=[hThB, winB[1], winB[2]], writes=[bank[1]])
        halo_c = ss[:, 7, 8:16].rearrange("p (f t) -> p f t", f=4)
        P.op("vector", lambda e: e.tensor_copy(out=halo_c, in_=ps[:, 1, 0:8].rearrange("p (f t) -> p f t", f=4)),
             reads=[bank[1]], writes=[ssb(6, 0)])
        P.op("vector", lambda e: e.tensor_tensor(out=zc[:, :, 0:2], in0=halo_c,
                                                 in1=ps[:, 1, 8:16].rearrange("p (f t) -> p f t", f=4), op=ALU.mult),
             reads=[bank[1], ssb(6, 0)], writes=zcB, strict=True)

        s_h2 = [P.dsem("s_h2_%d" % i) for i in range(2)]
        s_dbg = P.dsem("s_dbg")
        for blk in range(4):
            for jl in range(4):
                j = 4 * blk + jl
                P.op("scalar", lambda e, j=j: e.activation(out=htok[j % 2], in_=xres[:, j, :], func=AF.Square,
                                                           accum_out=ss[:, 0, j:j + 1]),
                     reads=[xresB[j]], writes=[htokB[j % 2], ssb(0, j)])
                rstd2(ss[:, 0, j:j + 1], ss[:, 1, j:j + 1], D, ssb(0, j), ssb(1, j))
                P.op("vector", lambda e, j=j: e.scalar_tensor_tensor(
                    out=htok[j % 2], in0=xres[:, j, :], scalar=ss[:, 1, j:j + 1], in1=gA[:],
                    op0=ALU.mult, op1=ALU.mult), reads=[xresB[j], ssb(1, j), gAB], writes=[htokB[j % 2]])
                P.op("tensor", tr_generic(htok[j % 2], 0, 8), reads=[htokB[j % 2], identB], writes=[bank[0]])
                P.op("scalar", lambda e, jl=jl: e.copy(out=hT[:, :, 128 * jl:128 * (jl + 1)], in_=bank_bf(0, 8)),
                     reads=[bank[0]], writes=[hTB[jl], tickB[blk][jl]])
            if blk < 3:
                load_x(blk + 1, after=[tickB[blk][0]])
            for fc in range(4):
                def mm_conv(e, fc=fc):
                    for bi, cg in ((2, 0), (3, 1), (4, 2)):
                        for k in range(KD):
                            i = e.matmul(ps[:, bi, :], lhsT=win_bf[cg][:, k, 128 * fc:128 * (fc + 1)],
                                         rhs=hT[:, k, :], start=(k == 0), stop=(k == KD - 1))
                    return i
                P.op("tensor", mm_conv, reads=hTB + [winB[0], winB[1], winB[2]], writes=[bank[2], bank[3], bank[4]])
                q2 = fc % 2
                P.op("scalar", lambda e, q2=q2: e.copy(out=c_sb[q2], in_=ps[:, 3, :]), reads=[bank[3]], writes=[c_sbB[q2]])
                P.op("vector", lambda e, fc=fc, q2=q2: e.tensor_tensor(out=zc[:, fc, 2:514], in0=c_sb[q2], in1=ps[:, 4, :],
                                                                       op=ALU.mult),
                     reads=[c_sbB[q2], bank[4]], writes=[zcB[fc]])
                P.op("scalar", lambda e, q2=q2: e.copy(out=b_sb[q2], in_=ps[:, 2, :]), reads=[bank[2]], writes=[b_sbB[q2]])
                P.op("scalar", lambda e, fc=fc, q2=q2: e.activation(out=acc[q2], in_=zc[:, fc, 2:514], func=AF.Copy,
                                                                    scale=wconvT[:, fc, 2:3]),
                     reads=[zcB[fc], wconvB], writes=[accB[q2]])

                P.op("vector", lambda e, fc=fc, q2=q2: e.scalar_tensor_tensor(
                    out=acc[q2], in0=zc[:, fc, 1:513], scalar=wconvT[:, fc, 1:2], in1=acc[q2], op0=ALU.mult, op1=ALU.add),
                    reads=[zcB[fc], wconvB], writes=[accB[q2]])
                P.op("vector", lambda e, fc=fc, q2=q2: e.scalar_tensor_tensor(
                    out=acc[q2], in0=zc[:, fc, 0:512], scalar=wconvT[:, fc, 0:1], in1=acc[q2], op0=ALU.mult, op1=ALU.add),
                    reads=[zcB[fc], wconvB], writes=[accB[q2]])
                P.op("gpsimd", lambda e, fc=fc: e.tensor_copy(out=zc[:, fc, 0:2], in_=zc[:, fc, 512:514]),
                     writes=[zcB[fc]])
                P.op("gpsimd", lambda e, fc=fc, q2=q2: e.tensor_tensor(out=yT[:, fc, :], in0=acc[q2], in1=b_sb[q2], op=ALU.mult),
                     reads=[accB[q2], b_sbB[q2]], writes=[yTcB[fc]])
            for jl in range(4):
                j = 4 * blk + jl
                cols = slice(128 * jl, 128 * (jl + 1))

                def mm_uv(e, cols=cols):
                    for bi, cg in ((5, 3), (6, 4)):
                        for k in range(KD):
                            i = e.matmul(ps[:, bi, :], lhsT=hT[:, k, cols], rhs=win_bf[cg][:, k, :],
                                         start=(k == 0), stop=(k == KD - 1))
                    return i
                P.op("tensor", mm_uv, reads=[hTB[jl], winB[3], winB[4]], writes=[bank[5], bank[6]])
                jp = j % 2
                P.op("scalar", lambda e, jp=jp: e.activation(out=gu[jp], in_=ps[:, 5, :], func=AF.Gelu_apprx_tanh),
                     reads=[bank[5]], writes=[guB[jp]])
                P.op("scalar", lambda e, j=j, jp=jp: e.activation(out=gv[jp], in_=ps[:, 6, :], func=AF.Gelu_apprx_tanh,
                                                                  accum_out=ss[:, 2, j:j + 1]),
                     reads=[bank[6]], writes=[gvB[jp], ssb(2, j)])

                P.op("gpsimd", lambda e, j=j: e.tensor_scalar(out=ss[:, 3, j:j + 1], in0=ss[:, 2, j:j + 1], scalar1=-1.0 / 512,
                                                              scalar2=0.0, op0=ALU.mult, op1=ALU.add),
                     reads=[ssb(2, j)], writes=[ssb(3, j)])
                P.op("gpsimd", lambda e, j=j: e.tensor_scalar(out=ss[:, 6, j:j + 1], in0=ss[:, 2, j:j + 1], scalar1=1.0 / 512,
                                                              scalar2=0.0, op0=ALU.mult, op1=ALU.add),
                     reads=[ssb(2, j)], writes=[ssb(6, 100 + j)])
                P.op("scalar", lambda e, j=j, jp=jp: e.activation(out=tmpv, in_=gv[jp], func=AF.Square,
                                                                  bias=ss[:, 3, j:j + 1], accum_out=ss[:, 4, j:j + 1]),
                     reads=[gvB[jp], ssb(3, j)], writes=[tmpvB, ssb(4, j)])
                rstd2(ss[:, 4, j:j + 1], ss[:, 5, j:j + 1], 512, ssb(4, j), ssb(5, j))
                P.op("vector", lambda e, j=j, jp=jp: e.scalar_tensor_tensor(
                    out=tmpv, in0=gv[jp], scalar=ss[:, 6, j:j + 1], in1=gsguB_t, op0=ALU.subtract, op1=ALU.mult),
                    reads=[gvB[jp], ssb(6, 100 + j), gsguB], writes=[tmpvB])
                P.op("scalar", lambda e, j=j: e.activation(out=vn, in_=tmpv, func=AF.Copy, scale=ss[:, 5, j:j + 1]),
                     reads=[tmpvB, ssb(5, j)], writes=[vnB])

                def mm_spatial(e):
                    e.matmul(ps[:, 7, :], lhsT=bhi, rhs=R_bf, start=True, stop=False)
                    e.matmul(ps[:, 7, :], lhsT=blo, rhs=R_bf, start=False, stop=False)
                    for h in range(8):
                        i = e.matmul(ps[:, 7, 64 * h:64 * (h + 1)], lhsT=wsT[:, h, :], rhs=vn[:, 64 * h:64 * (h + 1)],
                                     start=False, stop=(h == 7))
                    return i
                P.op("tensor", mm_spatial, reads=[vnB, wsTB, bsp2B, bsp4B, RB], writes=[bank[7]])
                P.op("vector", lambda e, jp=jp: e.tensor_tensor(out=ysg, in0=gu[jp], in1=ps[:, 7, :], op=ALU.mult),
                     reads=[guB[jp], bank[7]], writes=[ysgB])
                P.op("tensor", tr_generic(ysg, 1, 4), reads=[ysgB, identB], writes=[bank[1]])
                P.op("scalar", lambda e, cols=cols: e.copy(out=yT[:, 4:8, cols], in_=bank_bf(1, 4)),
                     reads=[bank[1]], writes=[yTsB[jl]])
            for jl in range(4):
                j = 4 * blk + jl
                cols = slice(128 * jl, 128 * (jl + 1))

                def mm_out(e, cols=cols):
                    for half in range(2):
                        for k in range(KD):
                            i = e.matmul(ps[:, 2 + half, :], lhsT=yT[:, k, cols], rhs=wout_bf[:, k, 512 * half:512 * (half + 1)],
                                         start=(k == 0), stop=(k == KD - 1))
                    return i
                P.op("tensor", mm_out, reads=yTcB + [yTsB[jl], woutB], writes=[bank[2], bank[3]])
                P.op("vector", lambda e, j=j: e.tensor_tensor(out=xres[:, j, :], in0=xres[:, j, :],
                                                              in1=ps[:, 2:4, :].rearrange("p a b -> p (a b)"), op=ALU.add),
                     reads=[bank[2], bank[3]], writes=[xresB[j]])
                if DEBUG:
                    P.op("sync", lambda e, j=j: e.dma_start(out=dbg_x2[128 * j:128 * (j + 1), :], in_=xres[:, j, :]),
                         reads=[xresB[j]], dma=s_dbg)
                    if jl == 0:
                        P.op("sync", lambda e, blk=blk: e.dma_start(out=dbg_yT[blk], in_=yT),
                             reads=yTcB + yTsB, dma=s_dbg)
                P.op("scalar", lambda e, j=j: e.activation(out=h2t[j % 2], in_=xres[:, j, :], func=AF.Square,
                                                           accum_out=ss[:, 0, j:j + 1]),
                     reads=[xresB[j]], writes=[h2tB[j % 2], ssb(0, j)])
                rstd2(ss[:, 0, j:j + 1], ss[:, 1, j:j + 1], D, ssb(0, j), ssb(1, j))
                P.op("vector", lambda e, j=j: e.scalar_tensor_tensor(
                    out=h2t[j % 2], in0=xres[:, j, :], scalar=ss[:, 1, j:j + 1], in1=gB_[:],
                    op0=ALU.mult, op1=ALU.mult), reads=[xresB[j], ssb(1, j), gBB], writes=[h2tB[j % 2]])
                P.op("sync", lambda e, j=j: e.dma_start(out=h2buf_d[128 * j:128 * (j + 1), :], in_=h2t[j % 2]),
                     reads=[h2tB[j % 2]], writes=[h2bufB[j]], dma=s_h2[j % 2])
                P.op("tensor", tr_generic(h2t[j % 2], 1, 8), reads=[h2tB[j % 2], identB], writes=[bank[1]])
                P.op("vector", lambda e, j=j: e.tensor_copy(out=h2T[j % 2], in_=bank_bf(1, 8)),
                     reads=[bank[1]], writes=[h2TB[j % 2]])

                def mm_router(e, j=j):
                    for k in range(KD):
                        i = e.matmul(ps[:, 4, 0:36], lhsT=h2T[j % 2][:, k, :], rhs=wr_bf[:, k, :],
                                     start=(k == 0), stop=(k == KD - 1))
                    return i
                P.op("tensor", mm_router, reads=[h2TB[j % 2], wrB], writes=[bank[4]])
                P.op("vector", lambda e, j=j: e.tensor_tensor(out=L[:, j, :], in0=ps[:, 4, 0:36], in1=biasB_t, op=ALU.add),
                     reads=[bank[4], biasB], pwrites=[LB])

        P.barrier()
        apos[0] = 0
        NH = 9
        h2s = [carve([128, D], BF16) for _ in range(3)]
        h2sB = [Buf("h2s%d" % i) for i in range(NH)]
        NW = 3
        wg_bf, wu_bf, wd_bf = [], [], []
        for w_ in range(NW):
            if w_ == NW - 1:
                route_start = apos[0]
            wg_bf.append(carve([128, KD, FE], BF16))
            wu_bf.append(carve([128, KD, FE], BF16))
            wd_bf.append(carve([128, 4, D], BF16))
        wslotB = [Buf("wslot%d" % i) for i in range(NW)]
        xe = [carve([128, NS, D], BF16) for _ in range(2)]
        xeB = [[Buf("xe%d_%d" % (i, s)) for s in range(NS)] for i in range(2)]
        xeT = [carve([128, KD, CAP], BF16) for _ in range(2)]
        xeTB = [[Buf("xeT%d_%d" % (i, s)) for s in range(NS)] for i in range(2)]
        hid = [carve([128, 4, CAP], BF16) for _ in range(2)]
        hidB = [[Buf("hid%d_%d" % (i, f)) for f in range(4)] for i in range(2)]
        sg = [carve([128, CAP], F32) for _ in range(2)]
        sgB = [Buf("sg%d" % i) for i in range(2)]
        ye = [carve([128, D], F32) for _ in range(3)]
        yeB = [Buf("ye%d" % i) for i in range(3)]
        for i in range(3):
            h2s.append(ye[i][:, 0:512].bitcast(BF16))
            h2s.append(ye[i][:, 512:1024].bitcast(BF16))
        moe_end = apos[0]
        assert moe_end <= ARENA, "MoE arena overflow %d" % moe_end
        apos[0] = route_start

        def rt(shape=(128, NT, 32), dt=F32):
            return carve(list(shape), dt)
        gmax = rt((128, NT))
        gsh = rt((128, NT, 4))
        gsum = rt((128, NT))
        gw = rt((128, NT))
        gmask = rt((128, NT, 4))
        em = rt()
        m1 = rt((128, NT))
        mask1 = rt()
        em2 = rt()
        m2 = rt((128, NT))
        mask2 = rt()
        dd = rt((128, NT))
        sel = rt((128, NT, 32), BF16)
        off = rt()
        pos = rt()
        ovf = rt()
        val = rt()
        slot = rt()
        t1 = rt()
        idxf = rt((128, NT))
        tot = rt((128, NE))
        scanA = rt((128, 16 + NE))
        scanB = rt((128, 16 + NE))
        eoff = rt((128, NE))
        assert apos[0] <= L_start, "routing tiles overflow %d" % apos[0]

        gl = L[:, :, 0:4]
        el = L[:, :, 4:36]

        def bc(a, n):
            return a.unsqueeze(2).to_broadcast([128, NT, n])

        def rop(eng, fn, reads=()):
            P.op(eng, fn, reads=list(reads), writes=[routeB], strict=True)

        rop("vector", lambda e: e.tensor_reduce(out=gmax, in_=gl, axis=AX.X, op=ALU.max), reads=[LB])
        rop("vector", lambda e: e.tensor_tensor(out=gsh, in0=gl, in1=bc(gmax, 4), op=ALU.subtract))
        rop("vector", lambda e: e.tensor_tensor(out=gmask, in0=gl, in1=bc(gmax, 4), op=ALU.is_equal))
        rop("vector", lambda e: e.tensor_scalar(out=gmask, in0=gmask, scalar1=BIG, scalar2=-BIG, op0=ALU.mult, op1=ALU.add))
        rop("vector", lambda e: e.tensor_tensor(out=em.rearrange("p j (g x) -> p j g x", g=4),
                                                in0=el.rearrange("p j (g x) -> p j g x", g=4),
                                                in1=gmask.unsqueeze(3).to_broadcast([128, NT, 4, 8]), op=ALU.add))
        rop("vector", lambda e: e.tensor_reduce(out=m1, in_=em, axis=AX.X, op=ALU.max))
        rop("vector", lambda e: e.tensor_tensor(out=mask1, in0=em, in1=bc(m1, 32), op=ALU.is_equal))
        rop("vector", lambda e: e.scalar_tensor_tensor(out=em2, in0=mask1, scalar=-BIG, in1=em, op0=ALU.mult, op1=ALU.add))
        rop("vector", lambda e: e.tensor_reduce(out=m2, in_=em2, axis=AX.X, op=ALU.max))
        rop("vector", lambda e: e.tensor_tensor(out=mask2, in0=em2, in1=bc(m2, 32), op=ALU.is_equal))
        rop("vector", lambda e: e.tensor_tensor(out=dd, in0=m2, in1=m1, op=ALU.subtract))
        rop("vector", lambda e: e.tensor_tensor(out=sel, in0=mask1, in1=mask2, op=ALU.add))

        rop("scalar", lambda e: e.activation(out=gsh, in_=gsh, func=AF.Exp))
        rop("scalar", lambda e: e.activation(out=dd, in_=dd, func=AF.Exp))
        def mm_pos(e):
            sel2 = sel.rearrange("p j x -> p (j x)")
            e.matmul(ps[:, 2, :], lhsT=ltri[:], rhs=sel2, start=True, stop=True)
            return e.matmul(ps[:, 3, :], lhsT=ones_bf[:], rhs=sel2, start=True, stop=True)
        P.op("tensor", mm_pos, reads=[routeB, constB, identB], writes=[bank[2], bank[3]])

        rop("vector", lambda e: e.tensor_reduce(out=gsum, in_=gsh, axis=AX.X, op=ALU.add))
        rop("vector", lambda e: e.reciprocal(out=gw, in_=gsum))
        rop("vector", lambda e: e.tensor_scalar(out=gsum, in0=dd, scalar1=1.0, scalar2=None, op0=ALU.add))
        rop("vector", lambda e: e.reciprocal(out=w1g[:], in_=gsum))
        rop("vector", lambda e: e.tensor_tensor(out=w1g[:], in0=w1g[:], in1=gw, op=ALU.mult))
        rop("vector", lambda e: e.tensor_tensor(out=w2g[:], in0=dd, in1=w1g[:], op=ALU.mult))
        cnt_sb = t1
        rop("vector", lambda e: e.tensor_copy(out=cnt_sb, in_=ps[:, 3, :].rearrange("p (j x) -> p j x", j=NT)), reads=[bank[3]])
        rop("vector", lambda e: e.memset(off[:, 0, :], 0.0))
        for j in range(1, NT):
            rop("vector", lambda e, j=j: e.tensor_tensor(out=off[:, j, :], in0=off[:, j - 1, :], in1=cnt_sb[:, j - 1, :], op=ALU.add))
        rop("vector", lambda e: e.tensor_tensor(out=tot, in0=off[:, NT - 1, :], in1=cnt_sb[:, NT - 1, :], op=ALU.add))
        rop("vector", lambda e: e.tensor_tensor(out=pos, in0=ps[:, 2, :].rearrange("p (j x) -> p j x", j=NT), in1=off, op=ALU.add),
            reads=[bank[2]])
        rop("vector", lambda e: e.tensor_scalar(out=ovf, in0=pos, scalar1=float(CAP), scalar2=1.0e6, op0=ALU.is_ge, op1=ALU.mult))
        rop("vector", lambda e: e.tensor_scalar(out=val, in0=pos, scalar1=float(CAP), scalar2=None, op0=ALU.is_lt))
        rop("vector", lambda e: e.memset(scanA, 0.0))
        rop("vector", lambda e: e.memset(scanB, 0.0))
        rop("vector", lambda e: e.tensor_scalar(out=tot, in0=tot, scalar1=float(CAP), scalar2=None, op0=ALU.min))
        rop("vector", lambda e: e.tensor_copy(out=scanA[:, 16:16 + NE], in_=tot))
        src_, dst_ = scanA, scanB
        for sh in (1, 2, 4, 8, 16):
            rop("vector", lambda e, src_=src_, dst_=dst_, sh=sh: e.tensor_tensor(
                out=dst_[:, 16:16 + NE], in0=src_[:, 16:16 + NE], in1=src_[:, 16 - sh:16 + NE - sh], op=ALU.add))
            src_, dst_ = dst_, src_
        rop("vector", lambda e, src_=src_: e.tensor_tensor(out=eoff, in0=src_[:, 16:16 + NE], in1=tot, op=ALU.subtract))
        rop("vector", lambda e: e.tensor_tensor(out=slot, in0=pos, in1=eoff.unsqueeze(1).to_broadcast([128, NT, NE]), op=ALU.add))
        rop("vector", lambda e: e.tensor_tensor(out=slot, in0=slot, in1=ovf, op=ALU.add))
        for (mk, idx, wk) in ((mask1, idx1, w1g), (mask2, idx2, w2g)):
            rop("vector", lambda e, mk=mk: e.tensor_tensor(out=t1, in0=mk, in1=slot, op=ALU.mult))
            rop("vector", lambda e: e.tensor_reduce(out=idxf, in_=t1, axis=AX.X, op=ALU.add))
            rop("vector", lambda e, idx=idx: e.tensor_copy(out=idx[:], in_=idxf))
            rop("vector", lambda e, mk=mk: e.tensor_tensor(out=t1, in0=mk, in1=val, op=ALU.mult))
            rop("vector", lambda e: e.tensor_reduce(out=idxf, in_=t1, axis=AX.X, op=ALU.add))
            rop("vector", lambda e, wk=wk: e.tensor_tensor(out=wk[:], in0=wk[:], in1=idxf, op=ALU.mult))

        srank = ovf.rearrange("p j x -> p (j x)")[:, 0:NE * NS].rearrange("p (e s) -> p e s", e=NE)
        sbase = val.rearrange("p j x -> p (j x)")[:, 0:NE * NS].rearrange("p (e s) -> p e s", e=NE)
        P.op("gpsimd", lambda e: e.iota(srank, pattern=[[0, NE], [128, NS]], base=0, channel_multiplier=1,
                                        allow_small_or_imprecise_dtypes=True), reads=[routeB], writes=[sidxB], strict=True)
        P.op("gpsimd", lambda e: e.iota(sbase, pattern=[[0, NE], [128, NS]], base=0, channel_multiplier=1,
                                        allow_small_or_imprecise_dtypes=True), reads=[routeB], writes=[sidxB], strict=True)
        P.op("vector", lambda e: e.tensor_tensor(out=sbase, in0=sbase, in1=eoff.unsqueeze(2).to_broadcast([128, NE, NS]), op=ALU.add),
             reads=[routeB], writes=[sidxB], strict=True)
        P.op("vector", lambda e: e.tensor_tensor(out=srank, in0=srank, in1=tot.unsqueeze(2).to_broadcast([128, NE, NS]),
                                                 op=ALU.is_ge), writes=[sidxB], strict=True)
        P.op("vector", lambda e: e.scalar_tensor_tensor(out=sbase, in0=srank, scalar=1.0e6, in1=sbase, op0=ALU.mult, op1=ALU.add),
             writes=[sidxB], strict=True)
        P.op("vector", lambda e: e.tensor_copy(out=sidx_i[:], in_=sbase), writes=[sidxB], strict=True)
        if DEBUG:
            for nm, apx in (("gmax", gmax), ("gsh", gsh), ("gmask", gmask), ("em", em), ("m1", m1), ("mask1", mask1),
                            ("em2", em2), ("m2", m2), ("mask2", mask2), ("dd", dd), ("off", off), ("pos", pos),
                            ("slot", slot), ("gw", gw), ("val", val)):
                dt_ = nc.dram_tensor("dbg_" + nm, list(apx.shape), F32, kind="ExternalOutput").ap()
                P.op("sync", lambda e, dt_=dt_, apx=apx: e.dma_start(out=dt_, in_=apx), reads=[routeB], dma=s_dbg)
            P.op("sync", lambda e: e.dma_start(out=dbg_L, in_=L), reads=[LB], dma=s_dbg)
            P.op("sync", lambda e: e.dma_start(out=dbg_i1, in_=idx1[:]), reads=[routeB], dma=s_dbg)
            P.op("sync", lambda e: e.dma_start(out=dbg_i2, in_=idx2[:]), reads=[routeB], dma=s_dbg)
            P.op("sync", lambda e: e.dma_start(out=dbg_w1, in_=w1g[:]), reads=[routeB], dma=s_dbg)
            P.op("sync", lambda e: e.dma_start(out=dbg_w2, in_=w2g[:]), reads=[routeB], dma=s_dbg)
        s_h2s = [P.dsem("s_h2s%d" % i) for i in range(NH)]
        s_scat = [P.dsem("s_scat%d" % i) for i in range(NH)]
        for j in range(NT):
            P.op("sync", lambda e, j=j: e.dma_start(out=h2s[j % NH], in_=h2buf_d[128 * j:128 * (j + 1), :]),
                 reads=[h2bufB[j]], writes=[h2sB[j % NH]], dma=s_h2s[j % NH])
            for idx in (idx1, idx2):
                P.op("gpsimd", lambda e, j=j, idx=idx: e.indirect_dma_start(
                    out=xbuf_d, out_offset=bass.IndirectOffsetOnAxis(ap=idx[:, j:j + 1], axis=0),
                    in_=h2s[j % NH], in_offset=None, bounds_check=bc_reg(e), oob_is_err=False),
                    reads=[h2sB[j % NH], routeB], pwrites=[xbufB], dma=s_scat[j % NH])

        s_w = [P.dsem("s_w%d" % i) for i in range(NW)]
        s_xe = [[P.dsem("s_xe%d_%d" % (i, s)) for s in range(NS)] for i in range(2)]
        s_ye = [P.dsem("s_ye%d" % i) for i in range(3)]
        for ex in range(NE):
            w = ex % NW
            s2 = ex % 2
            wdep = [routeB, sidxB] if w == NW - 1 else []
            P.op("gpsimd", lambda e, ex=ex, w=w: e.dma_start(
                out=wg_bf[w].rearrange("p k f -> p (k f)"), in_=wg_d[ex].rearrange("(p k) f -> p (k f)", p=128),
                max_dma_last_dim=8192), reads=wdep, pwrites=[wslotB[w]], dma=s_w[w])
            P.op("gpsimd", lambda e, ex=ex, w=w: e.dma_start(
                out=wu_bf[w].rearrange("p k f -> p (k f)"), in_=wu_d[ex].rearrange("(p k) f -> p (k f)", p=128),
                max_dma_last_dim=8192), reads=wdep, pwrites=[wslotB[w]], dma=s_w[w])
            P.op("gpsimd", lambda e, ex=ex, w=w: e.dma_start(
                out=wd_bf[w], in_=wd_d[ex].rearrange("(k p) f -> p k f", p=128)), reads=wdep, pwrites=[wslotB[w]], dma=s_w[w])
            for st in range(NS):
                P.op("gpsimd", lambda e, ex=ex, s2=s2, st=st: e.indirect_dma_start(
                    out=xe[s2][:, st, :], out_offset=None, in_=xbuf_d,
                    in_offset=bass.IndirectOffsetOnAxis(ap=sidx_i[:, ex, st:st + 1], axis=0),
                    bounds_check=bc_reg(e), oob_is_err=False),
                    reads=[xbufB, sidxB, routeB], writes=[xeB[s2][st]], dma=s_xe[s2][st], bscale=0.34)
            for st in range(NS):
                tb = st % 2
                def tr_strided(e, s2=s2, st=st, tb=tb):
                    for kk in range(KD):
                        i = e.transpose(out=bank_bf(tb, 8)[:, kk, :], in_=xe[s2][:, st, kk:D:KD], identity=ident[:])
                    return i
                P.op("tensor", tr_strided, reads=[xeB[s2][st], identB], writes=[bank[tb]])
                if st % 2 == 0:
                    P.op("scalar", lambda e, s2=s2, st=st, tb=tb: e.copy(out=xeT[s2][:, :, 128 * st:128 * (st + 1)], in_=bank_bf(tb, 8)),
                         reads=[bank[tb]], writes=[xeTB[s2][st]])
                else:
                    P.op("vector", lambda e, s2=s2, st=st, tb=tb: e.tensor_copy(out=xeT[s2][:, :, 128 * st:128 * (st + 1)], in_=bank_bf(tb, 8)),
                         reads=[bank[tb]], writes=[xeTB[s2][st]])
            for fc in range(4):
                bg = 2 + 2 * (fc % 2)
                bu = bg + 1

                def mm_gu(e, fc=fc, bg=bg, bu=bu, w=w, s2=s2):
                    for bi, wt in ((bg, wg_bf[w]), (bu, wu_bf[w])):
                        for k in range(KD):
                            i = e.matmul(ps[:, bi, 0:CAP], lhsT=wt[:, k, 128 * fc:128 * (fc + 1)],
                                         rhs=xeT[s2][:, k, :], start=(k == 0), stop=(k == KD - 1))
                    return i
                P.op("tensor", mm_gu, reads=[wslotB[w]] + xeTB[s2], writes=[bank[bg], bank[bu]])
                P.op("scalar", lambda e, bg=bg, fc=fc: e.activation(out=sg[fc % 2], in_=ps[:, bg, 0:CAP], func=AF.Silu),
                     reads=[bank[bg]], writes=[sgB[fc % 2]])
                P.op("vector", lambda e, bu=bu, fc=fc, s2=s2: e.tensor_tensor(
                    out=hid[s2][:, fc, :], in0=sg[fc % 2], in1=ps[:, bu, 0:CAP], op=ALU.mult),
                    reads=[sgB[fc % 2], bank[bu]], writes=[hidB[s2][fc]])
            for st in range(NS):
                yi = (ex * NS + st) % 3

                def mm_down(e, st=st, w=w, s2=s2):
                    for dh in range(2):
                        for fc in range(4):
                            i = e.matmul(ps[:, 6 + dh, :], lhsT=hid[s2][:, fc, 128 * st:128 * (st + 1)],
                                         rhs=wd_bf[w][:, fc, 512 * dh:512 * (dh + 1)], start=(fc == 0), stop=(fc == 3))
                    return i
                P.op("tensor", mm_down, reads=hidB[s2] + [wslotB[w]], writes=[bank[6], bank[7]])
                src = ps[:, 6:8, :].rearrange("p a b -> p (a b)")
                if (ex * NS + st) % 2 == 0:
                    P.op("scalar", lambda e, yi=yi, src=src: e.copy(out=ye[yi], in_=src),
                         reads=[bank[6], bank[7]], writes=[yeB[yi]])
                else:
                    P.op("vector", lambda e, yi=yi, src=src: e.tensor_copy(out=ye[yi], in_=src),
                         reads=[bank[6], bank[7]], writes=[yeB[yi]])
                P.op("gpsimd", lambda e, ex=ex, st=st, yi=yi: e.indirect_dma_start(
                    out=ybuf_d, out_offset=bass.IndirectOffsetOnAxis(ap=sidx_i[:, ex, st:st + 1], axis=0),
                    in_=ye[yi], in_offset=None, bounds_check=bc_reg(e), oob_is_err=False),
                    reads=[yeB[yi], sidxB], pwrites=[ybufB], dma=s_ye[yi], bscale=0.34)

        P.barrier()
        apos[0] = 0
        NY = 4
        NP3 = 3
        y1 = [carve([128, D], F32) for _ in range(NY)]
        y2 = [carve([128, D], F32) for _ in range(NY)]
        y1B = [Buf("y1_%d" % i) for i in range(NY)]
        y2B = [Buf("y2_%d" % i) for i in range(NY)]
        h3t = [carve([128, D], BF16) for _ in range(NP3)]
        h3tB = [Buf("h3t%d" % i) for i in range(NP3)]
        h3T = [carve([128, KD, 128], BF16) for _ in range(NP3)]
        h3TB = [Buf("h3T%d" % i) for i in range(NP3)]
        pT = [carve([128, 2, 128], BF16) for _ in range(NP3)]
        pTB = [Buf("pT%d" % i) for i in range(NP3)]
        sgm = [carve([128, D], F32) for _ in range(NP3)]
        sgmB = [Buf("sgm%d" % i) for i in range(NP3)]
        tmul = [carve([128, D], F32) for _ in range(NP3)]
        tmulB = [Buf("tmul%d" % i) for i in range(NP3)]
        ot = [carve([128, D], F32) for _ in range(NP3)]
        otB = [Buf("ot%d" % i) for i in range(NP3)]
        assert apos[0] <= ARENA
        ple_w0 = apos[0]
        wpg_bf = carve([128, KD, D], BF16)
        wpp_bf = carve([128, 2, D], BF16)
        p_bf = carve([128, NT, 256], BF16)
        assert apos[0] <= ARENA, "PLE weight prefetch region overflows the arena: %d" % apos[0]
        wpB, pbB = Buf("wple"), Buf("pbf")
        s_p3 = [P.dsem("s_p3_%d" % i) for i in range(4)]
        def ld_gple(e):
            with nc.allow_non_contiguous_dma(reason="gain vector as per-partition columns"):
                for k in range(KD):
                    i = e.dma_start(out=gplec[:, k:k + 1], in_=gple_d[0:1, 128 * k:128 * (k + 1)].rearrange("o p -> p o"))
                    if k < KD - 1:
                        i.then_inc(s_p3[0].h, 16)
            return i
        s_p3[0].n += 16 * (KD - 1)
        P.op("sync", ld_gple, writes=[gplecB], dma=s_p3[0])
        P.op("sync", lambda e: e.dma_start(out=gB_[:], in_=gfin_d.partition_broadcast(128)), writes=[gBB], dma=s_p3[1])
        P.op("gpsimd", lambda e: e.dma_start(out=wpg_bf, in_=wpg_d.rearrange("(k p) f -> p k f", p=128)),
             pwrites=[wpB], dma=s_p3[2])
        P.op("gpsimd", lambda e: e.dma_start(out=wpp_bf, in_=wpp_d.rearrange("(k p) f -> p k f", p=128)),
             pwrites=[wpB], dma=s_p3[2])
        P.op("gpsimd", lambda e: e.dma_start(out=p_bf, in_=p_d.rearrange("(j p) f -> p j f", p=128)),
             writes=[pbB], dma=s_p3[3])
        for k in range(KD):
            P.op("scalar", lambda e, k=k: e.activation(out=wpg_bf[:, k, :], in_=wpg_bf[:, k, :], func=AF.Copy, scale=gplec[:, k:k + 1]),
                 reads=[gplecB], writes=[wpB])


        s_y1 = [P.dsem("s_y1_%d" % i) for i in range(NY)]
        s_y2 = [P.dsem("s_y2_%d" % i) for i in range(NY)]
        s_out = [P.dsem("s_out%d" % i) for i in range(NP3)]
        out_ops = {}
        for j in range(NT):
            s2 = j % NP3
            b2 = j % 2
            sy = j % NY
            P.op("gpsimd", lambda e, j=j, sy=sy: e.indirect_dma_start(
                out=y1[sy], out_offset=None, in_=ybuf_d,
                in_offset=bass.IndirectOffsetOnAxis(ap=idx1[:, j:j + 1], axis=0),
                bounds_check=bc_reg(e), oob_is_err=False), reads=[ybufB, routeB], writes=[y1B[sy]], dma=s_y1[sy])
            P.op("gpsimd", lambda e, j=j, sy=sy: e.indirect_dma_start(
                out=y2[sy], out_offset=None, in_=ybuf_d,
                in_offset=bass.IndirectOffsetOnAxis(ap=idx2[:, j:j + 1], axis=0),
                bounds_check=bc_reg(e), oob_is_err=False), reads=[ybufB, routeB], writes=[y2B[sy]], dma=s_y2[sy])

            P.op("vector", lambda e, j=j, sy=sy: e.scalar_tensor_tensor(
                out=xres[:, j, :], in0=y1[sy], scalar=w1g[:, j:j + 1], in1=xres[:, j, :], op0=ALU.mult, op1=ALU.add),
                reads=[y1B[sy], routeB], writes=[xresB[j]])
            P.op("vector", lambda e, j=j, sy=sy: e.scalar_tensor_tensor(
                out=xres[:, j, :], in0=y2[sy], scalar=w2g[:, j:j + 1], in1=xres[:, j, :], op0=ALU.mult, op1=ALU.add),
                reads=[y2B[sy], routeB], writes=[xresB[j]])
            if DEBUG:
                P.op("sync", lambda e, j=j: e.dma_start(out=dbg_x3[128 * j:128 * (j + 1), :], in_=xres[:, j, :]),
                     reads=[xresB[j]], dma=s_dbg)
            P.op("scalar", lambda e, j=j, s2=s2: e.activation(out=h3t[s2], in_=xres[:, j, :], func=AF.Square,
                                                              accum_out=ss[:, 0, j:j + 1]),
                 reads=[xresB[j]], writes=[h3tB[s2], ssb(0, j)])
            rstd2(ss[:, 0, j:j + 1], ss[:, 1, j:j + 1], D, ssb(0, j), ssb(1, j))
            P.op("scalar", lambda e, j=j, s2=s2: e.activation(out=h3t[s2], in_=xres[:, j, :], func=AF.Copy, scale=ss[:, 1, j:j + 1]),
                 reads=[xresB[j], ssb(1, j)], writes=[h3tB[s2]])
            P.op("tensor", tr_generic(h3t[s2], b2, 8), reads=[h3tB[s2], identB], writes=[bank[b2]])
            P.op("scalar", lambda e, s2=s2, b2=b2: e.copy(out=h3T[s2], in_=bank_bf(b2, 8)), reads=[bank[b2]], writes=[h3TB[s2]])
            P.op("tensor", tr_generic(p_bf[:, j, :], 6 + b2, 2), reads=[pbB, identB], writes=[bank[6 + b2]])
            P.op("vector", lambda e, s2=s2, b2=b2: e.tensor_copy(out=pT[s2], in_=bank_bf(6 + b2, 2)), reads=[bank[6 + b2]], writes=[pTB[s2]])

            def mm_pg(e, s2=s2):
                for half in range(2):
                    for k in range(KD):
                        i = e.matmul(ps[:, 2 + half, :], lhsT=h3T[s2][:, k, :], rhs=wpg_bf[:, k, 512 * half:512 * (half + 1)],
                                     start=(k == 0), stop=(k == KD - 1))
                return i

            def mm_pp(e, s2=s2):
                for half in range(2):
                    for k in range(2):
                        i = e.matmul(ps[:, 4 + half, :], lhsT=pT[s2][:, k, :], rhs=wpp_bf[:, k, 512 * half:512 * (half + 1)],
                                     start=(k == 0), stop=(k == 1))
                return i
            P.op("tensor", mm_pg, reads=[h3TB[s2], wpB], writes=[bank[2], bank[3]])
            P.op("tensor", mm_pp, reads=[pTB[s2], wpB], writes=[bank[4], bank[5]])
            P.op("scalar", lambda e, s2=s2: e.activation(out=sgm[s2], in_=ps[:, 2:4, :].rearrange("p a b -> p (a b)"), func=AF.Sigmoid),
                 reads=[bank[2], bank[3]], writes=[sgmB[s2]])
            P.op("vector", lambda e, s2=s2: e.tensor_tensor(out=tmul[s2], in0=sgm[s2], in1=ps[:, 4:6, :].rearrange("p a b -> p (a b)"),
                                                            op=ALU.mult), reads=[sgmB[s2], bank[4], bank[5]], writes=[tmulB[s2]])
            P.op("gpsimd", lambda e, j=j, s2=s2: e.tensor_tensor(out=xres[:, j, :], in0=xres[:, j, :], in1=tmul[s2], op=ALU.add),
                 reads=[tmulB[s2]], writes=[xresB[j]])
            P.op("scalar", lambda e, j=j, s2=s2: e.activation(out=ot[s2], in_=xres[:, j, :], func=AF.Square,
                                                              accum_out=ss[:, 2, j:j + 1]),
                 reads=[xresB[j]], writes=[otB[s2], ssb(2, j)])
            rstd2(ss[:, 2, j:j + 1], ss[:, 3, j:j + 1], D, ssb(2, j), ssb(3, j))
            P.op("vector", lambda e, j=j, s2=s2: e.scalar_tensor_tensor(
                out=ot[s2], in0=xres[:, j, :], scalar=ss[:, 3, j:j + 1], in1=gB_[:], op0=ALU.mult, op1=ALU.mult),
                reads=[xresB[j], ssb(3, j), gBB], writes=[otB[s2]])
            out_ops[s2] = P.op("sync", lambda e, j=j, s2=s2: e.dma_start(out=out_d[128 * j:128 * (j + 1), :], in_=ot[s2]),
                               reads=[otB[s2]], dma=s_out[s2])
        P.wait_all("sync", list(out_ops.values()))

        block = es.enter_context(nc.Block())
        P.emit(block)
        build_nc.makespan = P.makespan
    return nc


_NC_CACHE = {}


def kernel(x, p, g_mix, w_in, w_conv, g_sgu, w_spatial, b_spatial, w_out, g_ffn,
           w_group, b_group, w_router, b_router, w_gate, w_up, w_down,
           g_ple, w_ple_gate, w_ple_proj, g_final):
    f = lambda a: np.ascontiguousarray(np.asarray(a, dtype=np.float32))
    x = f(x)
    p = f(p)
    shared = {
        "g_mix": f(g_mix).reshape(1, D),
        "w_in": f(w_in)[0],
        "w_conv": f(w_conv)[0],
        "g_sgu": f(g_sgu).reshape(1, 512),
        "w_spatial": f(w_spatial)[0],
        "b_spatial": f(b_spatial)[0],
        "w_out": f(w_out)[0],
        "g_ffn": f(g_ffn).reshape(1, D),
        "w_group": f(w_group)[0],
        "b_group": f(b_group).reshape(1, 4),
        "w_router": f(w_router)[0],
        "b_router": f(b_router).reshape(1, 32),
        "w_gate": f(w_gate)[0].reshape(NE, D, FE),
        "w_up": f(w_up)[0].reshape(NE, D, FE),
        "w_down": f(w_down)[0].reshape(NE, FE, D),
        "g_ple": f(g_ple).reshape(1, D),
        "w_ple_gate": f(w_ple_gate)[0],
        "w_ple_proj": f(w_ple_proj)[0],
        "g_final": f(g_final).reshape(1, D),
    }
    in_maps = []
    for c in range(NCORES):
        b, half = c // 2, c % 2
        t0 = half * TOK
        xc = x[b, t0:t0 + TOK]
        if half == 0:
            xh = np.zeros((2, D), np.float32)
        else:
            xh = x[b, t0 - 2:t0]
        m = dict(shared)
        m["x"] = np.ascontiguousarray(xc)
        m["xh"] = np.ascontiguousarray(xh)
        m["p"] = np.ascontiguousarray(p[0, b, t0:t0 + TOK])
        in_maps.append(m)
    if "nc" not in _NC_CACHE:
        _NC_CACHE["nc"] = build_nc()
    res = run_bass_kernel_spmd(_NC_CACHE["nc"], in_maps, core_ids=list(range(NCORES)))
    out = np.empty((4, 4096, D), np.float32)
    for c in range(NCORES):
        b, half = c // 2, c % 2
        out[b, half * TOK:(half + 1) * TOK] = res.results[c]["out"]
    return out
```

```python
import numpy as np
from contextlib import ExitStack

import concourse.bass as bass
import concourse.mybir as mybir
from concourse.bass_utils import run_bass_kernel_spmd

F32 = mybir.dt.float32
BF16 = mybir.dt.bfloat16
I32 = mybir.dt.int32
AF = mybir.ActivationFunctionType
ALU = mybir.AluOpType
AX = mybir.AxisListType

NCORES = 8
TOK = 2048
NT = 16
D = 1024
KD = 8
NE = 32
CAP = 384
NS = CAP // 128
FE = 512
EPS = 1e-6
BIG = 1.0e30
DEBUG = False
SYNC_ALL = True

ENGS = ("sync", "scalar", "vector", "gpsimd", "tensor")


class Sem:
    def __init__(self, h, step):
        self.h = h
        self.step = step
        self.n = 0
        self.last = None


class Buf:
    __slots__ = ("name", "w", "pw", "r")

    def __init__(self, name):
        self.name = name
        self.w = None
        self.pw = []
        self.r = []


class Op:
    __slots__ = ("eng", "fn", "dma", "target", "signal", "count", "deps", "odeps", "cidx", "pos", "strict",
                 "cost", "nbytes", "nleft", "kids", "okids", "ready", "fin", "iss")


DEF_COST = {"sync": 60.0, "scalar": 300.0, "vector": 250.0, "gpsimd": 500.0, "tensor": 300.0}
DMA_BW = 0.33e3
DMA_LAT = 2000.0


class _Dummy:
    def then_inc(self, *a, **k):
        return self


def _nfree(ap):
    n = 1
    for d in ap.shape[1:]:
        n *= int(d)
    return n


class CostProxy:
    def __init__(self, eng):
        self.eng = eng
        self.cost = 0.0
        self.nbytes = 0

    def to_reg(self, v):
        return None

    def __getattr__(self, name):
        def f(*args, **kw):
            out = kw.get("out", args[0] if args else None)
            eng = self.eng
            if name in ("dma_start", "indirect_dma_start"):
                src = kw.get("in_", args[1] if len(args) > 1 else out)
                n = min(_nfree(out) * int(out.shape[0]), _nfree(src) * int(src.shape[0]))
                self.nbytes += n * (2 if src.dtype == BF16 else 4)
                self.cost += {"sync": 80.0, "scalar": 80.0}.get(eng, 2500.0 if name.startswith("ind") else 1200.0)
            elif eng == "tensor":
                if name == "matmul":
                    rhs = kw.get("rhs", args[2] if len(args) > 2 else None)
                    self.cost += max(_nfree(rhs), 64) / 2.0 + 6.0
                else:
                    self.cost += 70.0
            elif eng == "scalar":
                self.cost += 224.0 + 0.833 * _nfree(out) + (60.0 if kw.get("accum_out") is not None else 0.0)
            elif eng == "vector":
                src = kw.get("in_", kw.get("in0", out))
                self.cost += 70.0 + 1.04 * max(_nfree(out), _nfree(src))
            else:
                self.cost += 300.0 + 2.0 * _nfree(out)
            return _Dummy()
        return f


class Prog:
    def __init__(self, nc, es):
        self.nc = nc
        self.es = es
        self.ops = []
        self.q = {e: [] for e in ENGS}
        self.esem = {e: Sem(es.enter_context(nc.semaphore("eng_" + e)), 1)
                     for e in ("scalar", "vector", "gpsimd", "tensor")}
        self.fence = {e: None for e in ENGS}
        self.since_fence = []

    def dsem(self, name):
        return Sem(self.es.enter_context(self.nc.semaphore(name)), 16)

    def _add(self, eng, fn, deps, dma, strict=False, cost=None, nbytes=0):
        o = Op()
        o.eng = eng
        o.fn = fn
        o.dma = dma
        o.signal = False
        o.count = None
        o.target = None
        o.strict = strict
        if cost is None and fn is not None:
            px = CostProxy(eng)
            fn(px)
            cost, nbytes = px.cost, px.nbytes
        o.cost = DEF_COST[eng] if cost is None else float(cost)
        o.nbytes = nbytes
        deps = list(deps)
        o.odeps = []
        if dma is not None:
            dma.n += 16
            o.target = dma.n
            if dma.last is not None:
                assert dma.last.eng == eng
                o.odeps.append(dma.last)
            dma.last = o
        if self.fence[eng] is not None:
            deps.append(self.fence[eng])
        seen = set()
        o.deps = []
        for d in deps:
            if id(d) not in seen:
                seen.add(id(d))
                o.deps.append(d)
        o.cidx = len(self.ops)
        self.ops.append(o)
        self.since_fence.append(o)
        return o

    def op(self, eng, fn, reads=(), writes=(), pwrites=(), dma=None, strict=False, cost=None, nbytes=0, bscale=1.0):
        deps = []
        for b in reads:
            if b.w is not None:
                deps.append(b.w)
            deps.extend(b.pw)
        for b in tuple(writes) + tuple(pwrites):
            if b.w is not None:
                deps.append(b.w)
            deps.extend(b.r)
        for b in writes:
            deps.extend(b.pw)
        o = self._add(eng, fn, deps, dma, strict, cost, nbytes)
        o.nbytes = o.nbytes * bscale
        for b in writes:
            b.w = o
            b.pw = []
            b.r = []
        for b in pwrites:
            b.pw.append(o)
        for b in reads:
            if b not in writes and b not in pwrites:
                b.r.append(o)
        return o

    def barrier(self):
        prev = list(self.since_fence)
        fs = {}
        for e in ENGS:
            fs[e] = self._add(e, None, prev, None, cost=0.0)
        self.fence = fs
        self.since_fence = []

    def wait_all(self, eng, ops):
        self._add(eng, None, list(ops), None, cost=0.0)

    def schedule(self):
        for o in self.ops:
            o.nleft = len(o.deps) + len(o.odeps)
            o.kids = []
            o.okids = []
            o.ready = 0.0
            o.fin = None
        for o in self.ops:
            for d in o.deps:
                d.kids.append(o)
            for d in o.odeps:
                d.okids.append(o)
        avail = {e: [] for e in ENGS}
        for o in self.ops:
            if o.nleft == 0:
                avail[o.eng].append(o)
        free = {e: 0.0 for e in ENGS}
        dma_free = 0.0
        n_left = len(self.ops)
        LOOK = 24
        while n_left:
            best = None
            for e in ENGS:
                av = avail[e]
                if not av:
                    continue
                av.sort(key=lambda o: o.cidx)
                cand = None
                for o in av[:LOOK]:
                    st = max(free[e], o.ready)
                    if cand is None or st < cand[0] - 1e-9:
                        cand = (st, o)
                if best is None or cand[0] < best[0] - 1e-9 or (abs(cand[0] - best[0]) <= 1e-9 and cand[1].cidx < best[1].cidx):
                    best = cand
            st, o = best
            e = o.eng
            avail[e].remove(o)
            free[e] = st + o.cost
            if o.dma is not None:
                dma_free = max(dma_free, st) + o.nbytes / DMA_BW
                o.fin = max(st + DMA_LAT, dma_free)
            else:
                o.fin = st + o.cost
            o.pos = len(self.q[e])
            self.q[e].append(o)
            n_left -= 1
            for k in o.kids:
                lat = 0.0
                if k.eng == o.eng and o.dma is None and k.dma is None and o.fn is not None:
                    lat = 1500.0 if k.strict else (400.0 if SYNC_ALL else 0.0)
                k.ready = max(k.ready, o.fin + lat)
                k.nleft -= 1
                if k.nleft == 0:
                    avail[k.eng].append(k)
            for k in o.okids:
                k.ready = max(k.ready, st + o.cost)
                k.nleft -= 1
                if k.nleft == 0:
                    avail[k.eng].append(k)
        self.makespan = max(o.fin for o in self.ops)

    def emit(self, block):
        self.schedule()
        for e in ENGS:
            for o in self.q[e]:
                best = {}
                for d in o.deps:
                    if d.fn is None and d.dma is None:
                        assert d.eng == o.eng
                        continue
                    if d.dma is None and o.dma is None and d.eng == o.eng and not (o.strict or SYNC_ALL):
                        assert d.pos < o.pos
                        continue
                    if d.dma is not None:
                        k = ("d", id(d.dma))
                        if k not in best or d.target > best[k].target:
                            best[k] = d
                    else:
                        k = ("e", d.eng)
                        if k not in best or d.pos > best[k].pos:
                            best[k] = d
                for d in o.odeps:
                    assert d.eng == o.eng and d.pos < o.pos
                o.deps = list(best.values())
                for d in o.deps:
                    if d.dma is None:
                        d.signal = True
        for e in ("scalar", "vector", "gpsimd", "tensor"):
            c = 0
            for o in self.q[e]:
                if o.dma is None and o.signal:
                    assert o.fn is not None
                    c += 1
                    o.count = c
        prog = self

        def run(eobj, ename):
            waited = {}
            for o in prog.q[ename]:
                for d in o.deps:
                    if d.dma is not None:
                        sem, val = d.dma.h, d.target
                    else:
                        sem, val = prog.esem[d.eng].h, d.count
                    k = id(sem)
                    if waited.get(k, 0) >= val:
                        continue
                    eobj.wait_ge(sem, val)
                    waited[k] = val
                if o.fn is None:
                    continue
                ins = o.fn(eobj)
                if o.dma is not None:
                    ins.then_inc(o.dma.h, 16)
                elif o.signal:
                    ins.then_inc(prog.esem[ename].h, 1)

        @block.sync
        def _(e):
            run(e, "sync")

        @block.scalar
        def _(e):
            run(e, "scalar")

        @block.vector
        def _(e):
            run(e, "vector")

        @block.gpsimd
        def _(e):
            run(e, "gpsimd")

        @block.tensor
        def _(e):
            run(e, "tensor")


def build_nc():
    nc = bass.Bass("TRN2", target_bir_lowering=False)

    def din(name, shape):
        return nc.dram_tensor(name, list(shape), F32, kind="ExternalInput").ap()

    x_d = din("x", [TOK, D])
    xh_d = din("xh", [2, D])
    p_d = din("p", [TOK, 256])
    gmix_d = din("g_mix", [1, D])
    win_d = din("w_in", [D, 2560])
    wconv_d = din("w_conv", [3, 512])
    gsgu_d = din("g_sgu", [1, 512])
    wsp_d = din("w_spatial", [8, 128, 128])
    bsp_d = din("b_spatial", [8, 128])
    wout_d = din("w_out", [D, D])
    gffn_d = din("g_ffn", [1, D])
    wgrp_d = din("w_group", [D, 4])
    bgrp_d = din("b_group", [1, 4])
    wrt_d = din("w_router", [4, D, 8])
    brt_d = din("b_router", [1, 32])
    wg_d = din("w_gate", [NE, D, FE])
    wu_d = din("w_up", [NE, D, FE])
    wd_d = din("w_down", [NE, FE, D])
    gple_d = din("g_ple", [1, D])
    wpg_d = din("w_ple_gate", [D, D])
    wpp_d = din("w_ple_proj", [256, D])
    gfin_d = din("g_final", [1, D])
    out_d = nc.dram_tensor("out", [TOK, D], F32, kind="ExternalOutput").ap()
    h2buf_d = nc.dram_tensor("h2buf", [TOK, D], BF16, kind="Internal").ap()
    xbuf_d = nc.dram_tensor("xbuf", [2 * TOK, D], BF16, kind="Internal").ap()
    ybuf_d = nc.dram_tensor("ybuf", [2 * TOK, D], F32, kind="Internal").ap()

    if DEBUG:
        dbg_x2 = nc.dram_tensor("dbg_x2", [TOK, D], F32, kind="ExternalOutput").ap()
        dbg_x3 = nc.dram_tensor("dbg_x3", [TOK, D], F32, kind="ExternalOutput").ap()
        dbg_L = nc.dram_tensor("dbg_L", [128, NT, 36], F32, kind="ExternalOutput").ap()
        dbg_i1 = nc.dram_tensor("dbg_i1", [128, NT], I32, kind="ExternalOutput").ap()
        dbg_i2 = nc.dram_tensor("dbg_i2", [128, NT], I32, kind="ExternalOutput").ap()
        dbg_w1 = nc.dram_tensor("dbg_w1", [128, NT], F32, kind="ExternalOutput").ap()
        dbg_w2 = nc.dram_tensor("dbg_w2", [128, NT], F32, kind="ExternalOutput").ap()
        dbg_yT = nc.dram_tensor("dbg_yT", [4, 128, KD, 512], BF16, kind="ExternalOutput").ap()

    with ExitStack() as es:
        def sb(name, shape, dt):
            return es.enter_context(nc.sbuf_tensor(name, list(shape), dt))

        P = Prog(nc, es)
        ps = es.enter_context(nc.psum_tensor("ps", [128, 8, 512], F32))
        bank = [Buf("bank%d" % i) for i in range(8)]

        def bank_bf(b, k):
            return ps[:, b, 0:64 * k].bitcast(BF16).rearrange("p (k t) -> p k t", k=k)

        xres = sb("xres", [128, NT, D], F32)
        xresB = [Buf("xres%d" % j) for j in range(NT)]
        gA = sb("gA", [128, D], F32)
        gB_ = sb("gB", [128, D], F32)
        gAB, gBB = Buf("gA"), Buf("gB")
        ident = sb("ident", [128, 128], BF16)
        ltri = sb("ltri", [128, 128], BF16)
        ones_bf = sb("ones_bf", [128, 128], BF16)
        onesf = sb("onesf", [128, 512], F32)
        mhalf = sb("mhalf", [128, 1], F32)
        gplec = sb("gplec", [128, KD], F32)
        gplecB = Buf("gplec")
        tickB = [[Buf("tick%d_%d" % (b, h)) for h in range(4)] for b in range(4)]
        constB = Buf("const")
        identB = Buf("ident")
        ss = sb("ss", [128, 8, NT], F32)
        _ssB = {}

        def ssb(row, j):
            if (row, j) not in _ssB:
                _ssB[(row, j)] = Buf("ss%d_%d" % (row, j))
            return _ssB[(row, j)]
        idx1 = sb("idx1", [128, NT], I32)
        idx2 = sb("idx2", [128, NT], I32)
        w1g = sb("w1g", [128, NT], F32)
        w2g = sb("w2g", [128, NT], F32)
        routeB = Buf("route")
        sidx_i = sb("sidx_i", [128, NE, NS], I32)
        sidxB = Buf("sidx")
        ARENA = 127 * 1024
        arena = sb("arena", [128, ARENA // 2], BF16)
        apos = [0]

        def carve(shape, dt):
            n = int(np.prod(shape[1:]))
            nb = n * (4 if dt in (F32, I32) else 2)
            nb = (nb + 31) // 32 * 32
            a = apos[0]
            assert a + nb <= ARENA, "arena overflow %d" % (a + nb)
            apos[0] = a + nb
            v = arena[0:shape[0], a // 2:(a + nb) // 2]
            if dt != BF16:
                v = v.bitcast(dt)
            v = v[:, 0:n]
            if len(shape) == 3:
                v = v.rearrange("p (a b) -> p a b", a=shape[1])
            elif len(shape) == 4:
                v = v.rearrange("p (a b c) -> p a b c", a=shape[1], b=shape[2])
            return v

        win_bf = [carve([128, KD, 512], BF16) for _ in range(5)]
        winB = [Buf("win%d" % i) for i in range(5)]
        wout_bf = carve([128, KD, D], BF16)
        woutB = Buf("wout")
        hT = carve([128, KD, 512], BF16)
        hTB = [Buf("hT%d" % i) for i in range(4)]
        htok = [carve([128, D], BF16) for _ in range(2)]
        htokB = [Buf("htok%d" % i) for i in range(2)]
        yT = carve([128, KD, 512], BF16)
        yTcB = [Buf("yTc%d" % i) for i in range(4)]
        yTsB = [Buf("yTs%d" % i) for i in range(4)]
        c_sb = [carve([128, 512], F32) for _ in range(2)]
        b_sb = [carve([128, 512], F32) for _ in range(2)]
        c_sbB = [Buf("c_sb%d" % i) for i in range(2)]
        b_sbB = [Buf("b_sb%d" % i) for i in range(2)]
        zc = carve([128, 4, 514], F32)
        zcB = [Buf("zc%d" % i) for i in range(4)]
        acc = [carve([128, 512], F32) for _ in range(2)]
        accB = [Buf("acc%d" % i) for i in range(2)]
        gu = [carve([128, 512], F32) for _ in range(2)]
        gv = [carve([128, 512], F32) for _ in range(2)]
        tmpv = carve([128, 512], F32)
        guB = [Buf("gu%d" % i) for i in range(2)]
        gvB = [Buf("gv%d" % i) for i in range(2)]
        tmpvB = Buf("tmpv")
        vn = carve([128, 512], BF16)
        ysg = carve([128, 512], BF16)
        vnB, ysgB = Buf("vn"), Buf("ysg")
        a0 = apos[0]
        xh_t = carve([128, D], F32)
        xhB = Buf("xh")
        hTh = carve([128, KD, 128], BF16)
        hThB = Buf("hTh")
        wsp_bf = carve([128, 8, 128], BF16)
        a1 = apos[0]
        apos[0] = a0
        h2t = [carve([128, D], BF16) for _ in range(2)]
        h2tB = [Buf("h2t%d" % i) for i in range(2)]
        h2T = [carve([128, KD, 128], BF16) for _ in range(2)]
        h2TB = [Buf("h2T%d" % i) for i in range(2)]
        apos[0] = max(apos[0], a1)
        wsT = carve([128, 8, 128], BF16)
        wspB, wsTB = Buf("wsp"), Buf("wsT")
        gsguB_t = carve([128, 512], F32)
        gsguB = Buf("gsgu")
        biasB_t = carve([128, 36], F32)
        biasB = Buf("bias")
        wconvT = carve([128, 4, 3], F32)
        wconvB = Buf("wconv")
        R_f = tmpv[0:8, :]
        R_bf = carve([8, 512], BF16)
        RB = Buf("R")
        bsp_f = carve([8, 128], F32)
        bsp_t = carve([8, 128], F32)
        bhi = carve([8, 128], BF16)
        blo = carve([8, 128], BF16)
        bspB = Buf("bsp")
        bsp4B = Buf("bsp4")
        wr_bf = carve([128, KD, 36], BF16)
        wrB = Buf("wr")
        mixer_end = apos[0]
        L_BYTES = NT * 36 * 4
        L_start = ARENA - L_BYTES
        assert mixer_end <= L_start, "mixer arena overflow %d" % mixer_end
        apos[0] = L_start
        L = carve([128, NT, 36], F32)
        LB = Buf("L")

        h2bufB = [Buf("h2buf%d" % j) for j in range(NT)]
        xbufB = Buf("xbuf")
        ybufB = Buf("ybuf")

        s_x = [P.dsem("s_x%d" % b) for b in range(4)]

        def load_x(b, after=()):
            P.op("sync", lambda e, b=b: e.dma_start(
                out=xres[:, 4 * b:4 * b + 4, :],
                in_=x_d[512 * b:512 * (b + 1), :].rearrange("(j p) d -> p j d", p=128)),
                reads=list(after), writes=xresB[4 * b:4 * b + 4], dma=s_x[b])
        load_x(0)
        s_c = [P.dsem("s_c%d" % i) for i in range(12)]
        P.op("sync", lambda e: e.dma_start(out=gA[:], in_=gmix_d.partition_broadcast(128)),
             writes=[gAB], dma=s_c[0])
        P.op("sync", lambda e: e.dma_start(out=gB_[:], in_=gffn_d.partition_broadcast(128)),
             writes=[gBB], dma=s_c[1])
        P.op("sync", lambda e: e.dma_start(out=gsguB_t, in_=gsgu_d.partition_broadcast(128)),
             writes=[gsguB], dma=s_c[2])
        P.op("sync", lambda e: e.dma_start(out=biasB_t[:, 0:4], in_=bgrp_d.partition_broadcast(128)),
             pwrites=[biasB], dma=s_c[3])
        P.op("sync", lambda e: e.dma_start(out=biasB_t[:, 4:36], in_=brt_d.partition_broadcast(128)),
             pwrites=[biasB], dma=s_c[3])

        def ld_wconv(e):
            with nc.allow_non_contiguous_dma(reason="tiny conv taps"):
                n = 0
                for k in range(3):
                    for fc in range(4):
                        i = e.dma_start(out=wconvT[:, fc, k:k + 1],
                                        in_=wconv_d[k:k + 1, 128 * fc:128 * (fc + 1)].rearrange("o p -> p o"))
                        n += 1
                        if n < 12:
                            i.then_inc(s_c[4].h, 16)
                return i
        s_c[4].n += 16 * 11
        P.op("sync", ld_wconv, writes=[wconvB], dma=s_c[4])
        P.op("sync", lambda e: e.dma_start(out=bsp_f, in_=bsp_d), writes=[bspB], dma=s_c[5])

        P.op("gpsimd", lambda e: e.memset(onesf[:], 1.0), writes=[constB])
        P.op("gpsimd", lambda e: e.memset(mhalf[:], -0.5), pwrites=[constB])


        P.op("gpsimd", lambda e: e.memset(ones_bf[:], 1.0), pwrites=[constB])
        P.op("gpsimd", lambda e: e.affine_select(out=ident[:], in_=onesf[:, 0:128], pattern=[[1, 128]],
                                                 compare_op=ALU.is_equal, fill=0.0, base=0, channel_multiplier=-1),
             reads=[constB], pwrites=[identB])
        P.op("gpsimd", lambda e: e.affine_select(out=ltri[:], in_=onesf[:, 0:128], pattern=[[1, 128]],
                                                 compare_op=ALU.is_gt, fill=0.0, base=0, channel_multiplier=-1),
             reads=[constB], pwrites=[identB])

        P.op("gpsimd", lambda e: e.affine_select(out=R_f, in_=onesf[0:8, :], pattern=[[1, 512]],
                                                 compare_op=ALU.is_ge, fill=0.0, base=0, channel_multiplier=-64),
             reads=[constB], writes=[tmpvB])
        P.op("gpsimd", lambda e: e.affine_select(out=R_bf, in_=R_f, pattern=[[-1, 512]],
                                                 compare_op=ALU.is_ge, fill=0.0, base=63, channel_multiplier=64),
             reads=[tmpvB], writes=[RB])

        s_win = [P.dsem("s_win%d" % i) for i in range(5)]
        s_w0 = [P.dsem("s_w0_%d" % i) for i in range(4)]
        P.op("gpsimd", lambda e: e.dma_start(out=wsp_bf, in_=wsp_d.rearrange("h t s -> t h s")),
             writes=[wspB], dma=s_w0[0])
        for cg in (1, 2, 0, 3, 4):
            P.op("gpsimd", lambda e, cg=cg: e.dma_start(
                out=win_bf[cg], in_=win_d[:, 512 * cg:512 * (cg + 1)].rearrange("(k p) f -> p k f", p=128)),
                writes=[winB[cg]], dma=s_win[cg])
        P.op("gpsimd", lambda e: e.dma_start(out=wout_bf, in_=wout_d.rearrange("(k p) f -> p k f", p=128)),
             writes=[woutB], dma=s_w0[1])

        def ld_wr(e):
            with nc.allow_non_contiguous_dma(reason="router weights"):
                e.dma_start(out=wr_bf[:, :, 0:4], in_=wgrp_d.rearrange("(k p) e -> p k e", p=128)).then_inc(s_w0[2].h, 16)
                for g in range(3):
                    e.dma_start(out=wr_bf[:, :, 4 + 8 * g:12 + 8 * g],
                                in_=wrt_d[g].rearrange("(k p) e -> p k e", p=128)).then_inc(s_w0[2].h, 16)
                return e.dma_start(out=wr_bf[:, :, 28:36], in_=wrt_d[3].rearrange("(k p) e -> p k e", p=128))
        s_w0[2].n += 64
        P.op("gpsimd", ld_wr, writes=[wrB], dma=s_w0[2])

        bsp2B, bsp3B = Buf("bsp2"), Buf("bsp3")
        P.op("vector", lambda e: e.tensor_copy(out=bhi, in_=bsp_f), reads=[bspB], writes=[bsp2B])
        P.op("vector", lambda e: e.tensor_tensor(out=bsp_t, in0=bsp_f, in1=bhi, op=ALU.subtract), reads=[bspB, bsp2B], writes=[bsp3B])
        P.op("vector", lambda e: e.tensor_copy(out=blo, in_=bsp_t), reads=[bsp3B], writes=[bsp4B])

        def tr_ws(e):
            for h in range(8):
                i = e.transpose(out=bank_bf(0, 8)[:, h, :], in_=wsp_bf[:, h, :], identity=ident[:])
            return i
        P.op("tensor", tr_ws, reads=[wspB, identB], writes=[bank[0]])
        P.op("vector", lambda e: e.tensor_copy(out=wsT, in_=bank_bf(0, 8)), reads=[bank[0]], writes=[wsTB])
        P.op("gpsimd", lambda e: e.affine_select(out=wsT, in_=wsT, pattern=[[0, 8], [1, 128]],
                                                 compare_op=ALU.is_ge, fill=0.0, base=0, channel_multiplier=-1),
             writes=[wsTB])

        _regs = {}

        def bc_reg(e):
            if isinstance(e, CostProxy):
                return None
            if "bc" not in _regs:
                _regs["bc"] = e.to_reg(2 * TOK - 1)
            return _regs["bc"]

        def rstd2(col_ss, col_out, n, rb, wb, eps=EPS):
            P.op("gpsimd", lambda e: e.tensor_scalar(out=col_out, in0=col_ss, scalar1=1.0 / n, scalar2=eps,
                                                     op0=ALU.mult, op1=ALU.add), reads=[rb], writes=[wb])
            P.op("gpsimd", lambda e: e.tensor_tensor(out=col_out, in0=col_out, in1=mhalf[:], op=ALU.pow),
                 reads=[constB], writes=[wb])

        P.op("gpsimd", lambda e: e.memset(xh_t, 0.0), writes=[xhB])
        s_xh = P.dsem("s_xh")
        P.op("sync", lambda e: e.dma_start(out=xh_t[0:2, :], in_=xh_d), writes=[xhB], dma=s_xh)
        P.op("scalar", lambda e: e.activation(out=htok[0], in_=xh_t, func=AF.Square, accum_out=ss[:, 7, 0:1]),
             reads=[xhB], writes=[htokB[0], ssb(7, 0)])
        rstd2(ss[:, 7, 0:1], ss[:, 7, 1:2], D, ssb(7, 0), ssb(7, 1))
        P.op("vector", lambda e: e.scalar_tensor_tensor(out=htok[0], in0=xh_t, scalar=ss[:, 7, 1:2], in1=gA[:],
                                                        op0=ALU.mult, op1=ALU.mult),
             reads=[xhB, ssb(7, 1), gAB], writes=[htokB[0]])

        def tr_generic(src, nb, k):
            def f(e):
                for kk in range(k):
                    i = e.transpose(out=bank_bf(nb, k)[:, kk, :], in_=src[:, 128 * kk:128 * (kk + 1)], identity=ident[:])
                return i
            return f
        P.op("tensor", tr_generic(htok[0], 0, 8), reads=[htokB[0], identB], writes=[bank[0]])
        P.op("scalar", lambda e: e.copy(out=hTh, in_=bank_bf(0, 8)), reads=[bank[0]], writes=[hThB])

        def mm_halo(e):
            for gi, cg in enumerate((1, 2)):
                for fc in range(4):
                    for k in range(KD):
                        i = e.matmul(ps[:, 1, (gi * 4 + fc) * 2:(gi * 4 + fc) * 2 + 2],
                                     lhsT=win_bf[cg][:, k, 128 * fc:128 * (fc + 1)], rhs=hTh[:, k, 0:2],
                                     start=(k == 0), stop=(k == KD - 1))
            return i
        P.op("tensor", mm_halo, reads=[hThB, winB[1], winB[2]], writes=[bank[1]])
        halo_c = ss[:, 7, 8:16].rearrange("p (f t) -> p f t", f=4)
        P.op("vector", lambda e: e.tensor_copy(out=halo_c, in_=ps[:, 1, 0:8].rearrange("p (f t) -> p f t", f=4)),
             reads=[bank[1]], writes=[ssb(6, 0)])
        P.op("vector", lambda e: e.tensor_tensor(out=zc[:, :, 0:2], in0=halo_c,
                                                 in1=ps[:, 1, 8:16].rearrange("p (f t) -> p f t", f=4), op=ALU.mult),
             reads=[bank[1], ssb(6, 0)], writes=zcB, strict=True)

        s_h2 = [P.dsem("s_h2_%d" % i) for i in range(2)]
        s_dbg = P.dsem("s_dbg")
        for blk in range(4):
            for jl in range(4):
                j = 4 * blk + jl
                P.op("scalar", lambda e, j=j: e.activation(out=htok[j % 2], in_=xres[:, j, :], func=AF.Square,
                                                           accum_out=ss[:, 0, j:j + 1]),
                     reads=[xresB[j]], writes=[htokB[j % 2], ssb(0, j)])
                rstd2(ss[:, 0, j:j + 1], ss[:, 1, j:j + 1], D, ssb(0, j), ssb(1, j))
                P.op("vector", lambda e, j=j: e.scalar_tensor_tensor(
                    out=htok[j % 2], in0=xres[:, j, :], scalar=ss[:, 1, j:j + 1], in1=gA[:],
                    op0=ALU.mult, op1=ALU.mult), reads=[xresB[j], ssb(1, j), gAB], writes=[htokB[j % 2]])
                P.op("tensor", tr_generic(htok[j % 2], 0, 8), reads=[htokB[j % 2], identB], writes=[bank[0]])
                P.op("scalar", lambda e, jl=jl: e.copy(out=hT[:, :, 128 * jl:128 * (jl + 1)], in_=bank_bf(0, 8)),
                     reads=[bank[0]], writes=[hTB[jl], tickB[blk][jl]])
            if blk < 3:
                load_x(blk + 1, after=[tickB[blk][0]])
            for fc in range(4):
                def mm_conv(e, fc=fc):
                    for bi, cg in ((2, 0), (3, 1), (4, 2)):
                        for k in range(KD):
                            i = e.matmul(ps[:, bi, :], lhsT=win_bf[cg][:, k, 128 * fc:128 * (fc + 1)],
                                         rhs=hT[:, k, :], start=(k == 0), stop=(k == KD - 1))
                    return i
                P.op("tensor", mm_conv, reads=hTB + [winB[0], winB[1], winB[2]], writes=[bank[2], bank[3], bank[4]])
                q2 = fc % 2
                P.op("scalar", lambda e, q2=q2: e.copy(out=c_sb[q2], in_=ps[:, 3, :]), reads=[bank[3]], writes=[c_sbB[q2]])
                P.op("vector", lambda e, fc=fc, q2=q2: e.tensor_tensor(out=zc[:, fc, 2:514], in0=c_sb[q2], in1=ps[:, 4, :],
                                                                       op=ALU.mult),
                     reads=[c_sbB[q2], bank[4]], writes=[zcB[fc]])
                P.op("scalar", lambda e, q2=q2: e.copy(out=b_sb[q2], in_=ps[:, 2, :]), reads=[bank[2]], writes=[b_sbB[q2]])
                P.op("scalar", lambda e, fc=fc, q2=q2: e.activation(out=acc[q2], in_=zc[:, fc, 2:514], func=AF.Copy,
                                                                    scale=wconvT[:, fc, 2:3]),
                     reads=[zcB[fc], wconvB], writes=[accB[q2]])

                P.op("vector", lambda e, fc=fc, q2=q2: e.scalar_tensor_tensor(
                    out=acc[q2], in0=zc[:, fc, 1:513], scalar=wconvT[:, fc, 1:2], in1=acc[q2], op0=ALU.mult, op1=ALU.add),
                    reads=[zcB[fc], wconvB], writes=[accB[q2]])
                P.op("vector", lambda e, fc=fc, q2=q2: e.scalar_tensor_tensor(
                    out=acc[q2], in0=zc[:, fc, 0:512], scalar=wconvT[:, fc, 0:1], in1=acc[q2], op0=ALU.mult, op1=ALU.add),
                    reads=[zcB[fc], wconvB], writes=[accB[q2]])
                P.op("gpsimd", lambda e, fc=fc: e.tensor_copy(out=zc[:, fc, 0:2], in_=zc[:, fc, 512:514]),
                     writes=[zcB[fc]])
                P.op("gpsimd", lambda e, fc=fc, q2=q2: e.tensor_tensor(out=yT[:, fc, :], in0=acc[q2], in1=b_sb[q2], op=ALU.mult),
                     reads=[accB[q2], b_sbB[q2]], writes=[yTcB[fc]])
            for jl in range(4):
                j = 4 * blk + jl
                cols = slice(128 * jl, 128 * (jl + 1))

                def mm_uv(e, cols=cols):
                    for bi, cg in ((5, 3), (6, 4)):
                        for k in range(KD):
                            i = e.matmul(ps[:, bi, :], lhsT=hT[:, k, cols], rhs=win_bf[cg][:, k, :],
                                         start=(k == 0), stop=(k == KD - 1))
                    return i
                P.op("tensor", mm_uv, reads=[hTB[jl], winB[3], winB[4]], writes=[bank[5], bank[6]])
                jp = j % 2
                P.op("scalar", lambda e, jp=jp: e.activation(out=gu[jp], in_=ps[:, 5, :], func=AF.Gelu_apprx_tanh),
                     reads=[bank[5]], writes=[guB[jp]])
                P.op("scalar", lambda e, j=j, jp=jp: e.activation(out=gv[jp], in_=ps[:, 6, :], func=AF.Gelu_apprx_tanh,
                                                                  accum_out=ss[:, 2, j:j + 1]),
                     reads=[bank[6]], writes=[gvB[jp], ssb(2, j)])

                P.op("gpsimd", lambda e, j=j: e.tensor_scalar(out=ss[:, 3, j:j + 1], in0=ss[:, 2, j:j + 1], scalar1=-1.0 / 512,
                                                              scalar2=0.0, op0=ALU.mult, op1=ALU.add),
                     reads=[ssb(2, j)], writes=[ssb(3, j)])
                P.op("gpsimd", lambda e, j=j: e.tensor_scalar(out=ss[:, 6, j:j + 1], in0=ss[:, 2, j:j + 1], scalar1=1.0 / 512,
                                                              scalar2=0.0, op0=ALU.mult, op1=ALU.add),
                     reads=[ssb(2, j)], writes=[ssb(6, 100 + j)])
                P.op("scalar", lambda e, j=j, jp=jp: e.activation(out=tmpv, in_=gv[jp], func=AF.Square,
                                                                  bias=ss[:, 3, j:j + 1], accum_out=ss[:, 4, j:j + 1]),
                     reads=[gvB[jp], ssb(3, j)], writes=[tmpvB, ssb(4, j)])
                rstd2(ss[:, 4, j:j + 1], ss[:, 5, j:j + 1], 512, ssb(4, j), ssb(5, j))
                P.op("vector", lambda e, j=j, jp=jp: e.scalar_tensor_tensor(
                    out=tmpv, in0=gv[jp], scalar=ss[:, 6, j:j + 1], in1=gsguB_t, op0=ALU.subtract, op1=ALU.mult),
                    reads=[gvB[jp], ssb(6, 100 + j), gsguB], writes=[tmpvB])
                P.op("scalar", lambda e, j=j: e.activation(out=vn, in_=tmpv, func=AF.Copy, scale=ss[:, 5, j:j + 1]),
                     reads=[tmpvB, ssb(5, j)], writes=[vnB])

                def mm_spatial(e):
                    e.matmul(ps[:, 7, :], lhsT=bhi, rhs=R_bf, start=True, stop=False)
                    e.matmul(ps[:, 7, :], lhsT=blo, rhs=R_bf, start=False, stop=False)
                    for h in range(8):
                        i = e.matmul(ps[:, 7, 64 * h:64 * (h + 1)], lhsT=wsT[:, h, :], rhs=vn[:, 64 * h:64 * (h + 1)],
                                     start=False, stop=(h == 7))
                    return i
                P.op("tensor", mm_spatial, reads=[vnB, wsTB, bsp2B, bsp4B, RB], writes=[bank[7]])
                P.op("vector", lambda e, jp=jp: e.tensor_tensor(out=ysg, in0=gu[jp], in1=ps[:, 7, :], op=ALU.mult),
                     reads=[guB[jp], bank[7]], writes=[ysgB])
                P.op("tensor", tr_generic(ysg, 1, 4), reads=[ysgB, identB], writes=[bank[1]])
                P.op("scalar", lambda e, cols=cols: e.copy(out=yT[:, 4:8, cols], in_=bank_bf(1, 4)),
                     reads=[bank[1]], writes=[yTsB[jl]])
            for jl in range(4):
                j = 4 * blk + jl
                cols = slice(128 * jl, 128 * (jl + 1))

                def mm_out(e, cols=cols):
                    for half in range(2):
                        for k in range(KD):
                            i = e.matmul(ps[:, 2 + half, :], lhsT=yT[:, k, cols], rhs=wout_bf[:, k, 512 * half:512 * (half + 1)],
                                         start=(k == 0), stop=(k == KD - 1))
                    return i
                P.op("tensor", mm_out, reads=yTcB + [yTsB[jl], woutB], writes=[bank[2], bank[3]])
                P.op("vector", lambda e, j=j: e.tensor_tensor(out=xres[:, j, :], in0=xres[:, j, :],
                                                              in1=ps[:, 2:4, :].rearrange("p a b -> p (a b)"), op=ALU.add),
                     reads=[bank[2], bank[3]], writes=[xresB[j]])
                if DEBUG:
                    P.op("sync", lambda e, j=j: e.dma_start(out=dbg_x2[128 * j:128 * (j + 1), :], in_=xres[:, j, :]),
                         reads=[xresB[j]], dma=s_dbg)
                    if jl == 0:
                        P.op("sync", lambda e, blk=blk: e.dma_start(out=dbg_yT[blk], in_=yT),
                             reads=yTcB + yTsB, dma=s_dbg)
                P.op("scalar", lambda e, j=j: e.activation(out=h2t[j % 2], in_=xres[:, j, :], func=AF.Square,
                                                           accum_out=ss[:, 0, j:j + 1]),
                     reads=[xresB[j]], writes=[h2tB[j % 2], ssb(0, j)])
                rstd2(ss[:, 0, j:j + 1], ss[:, 1, j:j + 1], D, ssb(0, j), ssb(1, j))
                P.op("vector", lambda e, j=j: e.scalar_tensor_tensor(
                    out=h2t[j % 2], in0=xres[:, j, :], scalar=ss[:, 1, j:j + 1], in1=gB_[:],
                    op0=ALU.mult, op1=ALU.mult), reads=[xresB[j], ssb(1, j), gBB], writes=[h2tB[j % 2]])
                P.op("sync", lambda e, j=j: e.dma_start(out=h2buf_d[128 * j:128 * (j + 1), :], in_=h2t[j % 2]),
                     reads=[h2tB[j % 2]], writes=[h2bufB[j]], dma=s_h2[j % 2])
                P.op("tensor", tr_generic(h2t[j % 2], 1, 8), reads=[h2tB[j % 2], identB], writes=[bank[1]])
                P.op("vector", lambda e, j=j: e.tensor_copy(out=h2T[j % 2], in_=bank_bf(1, 8)),
                     reads=[bank[1]], writes=[h2TB[j % 2]])

                def mm_router(e, j=j):
                    for k in range(KD):
                        i = e.matmul(ps[:, 4, 0:36], lhsT=h2T[j % 2][:, k, :], rhs=wr_bf[:, k, :],
                                     start=(k == 0), stop=(k == KD - 1))
                    return i
                P.op("tensor", mm_router, reads=[h2TB[j % 2], wrB], writes=[bank[4]])
                P.op("vector", lambda e, j=j: e.tensor_tensor(out=L[:, j, :], in0=ps[:, 4, 0:36], in1=biasB_t, op=ALU.add),
                     reads=[bank[4], biasB], pwrites=[LB])

        P.barrier()
        apos[0] = 0
        NH = 9
        h2s = [carve([128, D], BF16) for _ in range(3)]
        h2sB = [Buf("h2s%d" % i) for i in range(NH)]
        NW = 3
        wg_bf, wu_bf, wd_bf = [], [], []
        for w_ in range(NW):
            if w_ == NW - 1:
                route_start = apos[0]
            wg_bf.append(carve([128, KD, FE], BF16))
            wu_bf.append(carve([128, KD, FE], BF16))
            wd_bf.append(carve([128, 4, D], BF16))
        wslotB = [Buf("wslot%d" % i) for i in range(NW)]
        xe = [carve([128, NS, D], BF16) for _ in range(2)]
        xeB = [[Buf("xe%d_%d" % (i, s)) for s in range(NS)] for i in range(2)]
        xeT = [carve([128, KD, CAP], BF16) for _ in range(2)]
        xeTB = [[Buf("xeT%d_%d" % (i, s)) for s in range(NS)] for i in range(2)]
        hid = [carve([128, 4, CAP], BF16) for _ in range(2)]
        hidB = [[Buf("hid%d_%d" % (i, f)) for f in range(4)] for i in range(2)]
        sg = [carve([128, CAP], F32) for _ in range(2)]
        sgB = [Buf("sg%d" % i) for i in range(2)]
        ye = [carve([128, D], F32) for _ in range(3)]
        yeB = [Buf("ye%d" % i) for i in range(3)]
        for i in range(3):
            h2s.append(ye[i][:, 0:512].bitcast(BF16))
            h2s.append(ye[i][:, 512:1024].bitcast(BF16))
        moe_end = apos[0]
        assert moe_end <= ARENA, "MoE arena overflow %d" % moe_end
        apos[0] = route_start

        def rt(shape=(128, NT, 32), dt=F32):
            return carve(list(shape), dt)
        gmax = rt((128, NT))
        gsh = rt((128, NT, 4))
        gsum = rt((128, NT))
        gw = rt((128, NT))
        gmask = rt((128, NT, 4))
        em = rt()
        m1 = rt((128, NT))
        mask1 = rt()
        em2 = rt()
        m2 = rt((128, NT))
        mask2 = rt()
        dd = rt((128, NT))
        sel = rt((128, NT, 32), BF16)
        off = rt()
        pos = rt()
        ovf = rt()
        val = rt()
        slot = rt()
        t1 = rt()
        idxf = rt((128, NT))
        tot = rt((128, NE))
        scanA = rt((128, 16 + NE))
        scanB = rt((128, 16 + NE))
        eoff = rt((128, NE))
        assert apos[0] <= L_start, "routing tiles overflow %d" % apos[0]

        gl = L[:, :, 0:4]
        el = L[:, :, 4:36]

        def bc(a, n):
            return a.unsqueeze(2).to_broadcast([128, NT, n])

        def rop(eng, fn, reads=()):
            P.op(eng, fn, reads=list(reads), writes=[routeB], strict=True)

        rop("vector", lambda e: e.tensor_reduce(out=gmax, in_=gl, axis=AX.X, op=ALU.max), reads=[LB])
        rop("vector", lambda e: e.tensor_tensor(out=gsh, in0=gl, in1=bc(gmax, 4), op=ALU.subtract))
        rop("vector", lambda e: e.tensor_tensor(out=gmask, in0=gl, in1=bc(gmax, 4), op=ALU.is_equal))
        rop("vector", lambda e: e.tensor_scalar(out=gmask, in0=gmask, scalar1=BIG, scalar2=-BIG, op0=ALU.mult, op1=ALU.add))
        rop("vector", lambda e: e.tensor_tensor(out=em.rearrange("p j (g x) -> p j g x", g=4),
                                                in0=el.rearrange("p j (g x) -> p j g x", g=4),
                                                in1=gmask.unsqueeze(3).to_broadcast([128, NT, 4, 8]), op=ALU.add))
        rop("vector", lambda e: e.tensor_reduce(out=m1, in_=em, axis=AX.X, op=ALU.max))
        rop("vector", lambda e: e.tensor_tensor(out=mask1, in0=em, in1=bc(m1, 32), op=ALU.is_equal))
        rop("vector", lambda e: e.scalar_tensor_tensor(out=em2, in0=mask1, scalar=-BIG, in1=em, op0=ALU.mult, op1=ALU.add))
        rop("vector", lambda e: e.tensor_reduce(out=m2, in_=em2, axis=AX.X, op=ALU.max))
        rop("vector", lambda e: e.tensor_tensor(out=mask2, in0=em2, in1=bc(m2, 32), op=ALU.is_equal))
        rop("vector", lambda e: e.tensor_tensor(out=dd, in0=m2, in1=m1, op=ALU.subtract))
        rop("vector", lambda e: e.tensor_tensor(out=sel, in0=mask1, in1=mask2, op=ALU.add))

        rop("scalar", lambda e: e.activation(out=gsh, in_=gsh, func=AF.Exp))
        rop("scalar", lambda e: e.activation(out=dd, in_=dd, func=AF.Exp))
        def mm_pos(e):
            sel2 = sel.rearrange("p j x -> p (j x)")
            e.matmul(ps[:, 2, :], lhsT=ltri[:], rhs=sel2, start=True, stop=True)
            return e.matmul(ps[:, 3, :], lhsT=ones_bf[:], rhs=sel2, start=True, stop=True)
        P.op("tensor", mm_pos, reads=[routeB, constB, identB], writes=[bank[2], bank[3]])

        rop("vector", lambda e: e.tensor_reduce(out=gsum, in_=gsh, axis=AX.X, op=ALU.add))
        rop("vector", lambda e: e.reciprocal(out=gw, in_=gsum))
        rop("vector", lambda e: e.tensor_scalar(out=gsum, in0=dd, scalar1=1.0, scalar2=None, op0=ALU.add))
        rop("vector", lambda e: e.reciprocal(out=w1g[:], in_=gsum))
        rop("vector", lambda e: e.tensor_tensor(out=w1g[:], in0=w1g[:], in1=gw, op=ALU.mult))
        rop("vector", lambda e: e.tensor_tensor(out=w2g[:], in0=dd, in1=w1g[:], op=ALU.mult))
        cnt_sb = t1
        rop("vector", lambda e: e.tensor_copy(out=cnt_sb, in_=ps[:, 3, :].rearrange("p (j x) -> p j x", j=NT)), reads=[bank[3]])
        rop("vector", lambda e: e.memset(off[:, 0, :], 0.0))
        for j in range(1, NT):
            rop("vector", lambda e, j=j: e.tensor_tensor(out=off[:, j, :], in0=off[:, j - 1, :], in1=cnt_sb[:, j - 1, :], op=ALU.add))
        rop("vector", lambda e: e.tensor_tensor(out=tot, in0=off[:, NT - 1, :], in1=cnt_sb[:, NT - 1, :], op=ALU.add))
        rop("vector", lambda e: e.tensor_tensor(out=pos, in0=ps[:, 2, :].rearrange("p (j x) -> p j x", j=NT), in1=off, op=ALU.add),
            reads=[bank[2]])
        rop("vector", lambda e: e.tensor_scalar(out=ovf, in0=pos, scalar1=float(CAP), scalar2=1.0e6, op0=ALU.is_ge, op1=ALU.mult))
        rop("vector", lambda e: e.tensor_scalar(out=val, in0=pos, scalar1=float(CAP), scalar2=None, op0=ALU.is_lt))
        rop("vector", lambda e: e.memset(scanA, 0.0))
        rop("vector", lambda e: e.memset(scanB, 0.0))
        rop("vector", lambda e: e.tensor_scalar(out=tot, in0=tot, scalar1=float(CAP), scalar2=None, op0=ALU.min))
        rop("vector", lambda e: e.tensor_copy(out=scanA[:, 16:16 + NE], in_=tot))
        src_, dst_ = scanA, scanB
        for sh in (1, 2, 4, 8, 16):
            rop("vector", lambda e, src_=src_, dst_=dst_, sh=sh: e.tensor_tensor(
                out=dst_[:, 16:16 + NE], in0=src_[:, 16:16 + NE], in1=src_[:, 16 - sh:16 + NE - sh], op=ALU.add))
            src_, dst_ = dst_, src_
        rop("vector", lambda e, src_=src_: e.tensor_tensor(out=eoff, in0=src_[:, 16:16 + NE], in1=tot, op=ALU.subtract))
        rop("vector", lambda e: e.tensor_tensor(out=slot, in0=pos, in1=eoff.unsqueeze(1).to_broadcast([128, NT, NE]), op=ALU.add))
        rop("vector", lambda e: e.tensor_tensor(out=slot, in0=slot, in1=ovf, op=ALU.add))
        for (mk, idx, wk) in ((mask1, idx1, w1g), (mask2, idx2, w2g)):
            rop("vector", lambda e, mk=mk: e.tensor_tensor(out=t1, in0=mk, in1=slot, op=ALU.mult))
            rop("vector", lambda e: e.tensor_reduce(out=idxf, in_=t1, axis=AX.X, op=ALU.add))
            rop("vector", lambda e, idx=idx: e.tensor_copy(out=idx[:], in_=idxf))
            rop("vector", lambda e, mk=mk: e.tensor_tensor(out=t1, in0=mk, in1=val, op=ALU.mult))
            rop("vector", lambda e: e.tensor_reduce(out=idxf, in_=t1, axis=AX.X, op=ALU.add))
            rop("vector", lambda e, wk=wk: e.tensor_tensor(out=wk[:], in0=wk[:], in1=idxf, op=ALU.mult))

        srank = ovf.rearrange("p j x -> p (j x)")[:, 0:NE * NS].rearrange("p (e s) -> p e s", e=NE)
        sbase = val.rearrange("p j x -> p (j x)")[:, 0:NE * NS].rearrange("p (e s) -> p e s", e=NE)
        P.op("gpsimd", lambda e: e.iota(srank, pattern=[[0, NE], [128, NS]], base=0, channel_multiplier=1,
                                        allow_small_or_imprecise_dtypes=True), reads=[routeB], writes=[sidxB], strict=True)
        P.op("gpsimd", lambda e: e.iota(sbase, pattern=[[0, NE], [128, NS]], base=0, channel_multiplier=1,
                                        allow_small_or_imprecise_dtypes=True), reads=[routeB], writes=[sidxB], strict=True)
        P.op("vector", lambda e: e.tensor_tensor(out=sbase, in0=sbase, in1=eoff.unsqueeze(2).to_broadcast([128, NE, NS]), op=ALU.add),
             reads=[routeB], writes=[sidxB], strict=True)
        P.op("vector", lambda e: e.tensor_tensor(out=srank, in0=srank, in1=tot.unsqueeze(2).to_broadcast([128, NE, NS]),
                                                 op=ALU.is_ge), writes=[sidxB], strict=True)
        P.op("vector", lambda e: e.scalar_tensor_tensor(out=sbase, in0=srank, scalar=1.0e6, in1=sbase, op0=ALU.mult, op1=ALU.add),
             writes=[sidxB], strict=True)
        P.op("vector", lambda e: e.tensor_copy(out=sidx_i[:], in_=sbase), writes=[sidxB], strict=True)
        if DEBUG:
            for nm, apx in (("gmax", gmax), ("gsh", gsh), ("gmask", gmask), ("em", em), ("m1", m1), ("mask1", mask1),
                            ("em2", em2), ("m2", m2), ("mask2", mask2), ("dd", dd), ("off", off), ("pos", pos),
                            ("slot", slot), ("gw", gw), ("val", val)):
                dt_ = nc.dram_tensor("dbg_" + nm, list(apx.shape), F32, kind="ExternalOutput").ap()
                P.op("sync", lambda e, dt_=dt_, apx=apx: e.dma_start(out=dt_, in_=apx), reads=[routeB], dma=s_dbg)
            P.op("sync", lambda e: e.dma_start(out=dbg_L, in_=L), reads=[LB], dma=s_dbg)
            P.op("sync", lambda e: e.dma_start(out=dbg_i1, in_=idx1[:]), reads=[routeB], dma=s_dbg)
            P.op("sync", lambda e: e.dma_start(out=dbg_i2, in_=idx2[:]), reads=[routeB], dma=s_dbg)
            P.op("sync", lambda e: e.dma_start(out=dbg_w1, in_=w1g[:]), reads=[routeB], dma=s_dbg)
            P.op("sync", lambda e: e.dma_start(out=dbg_w2, in_=w2g[:]), reads=[routeB], dma=s_dbg)
        s_h2s = [P.dsem("s_h2s%d" % i) for i in range(NH)]
        s_scat = [P.dsem("s_scat%d" % i) for i in range(NH)]
        for j in range(NT):
            P.op("sync", lambda e, j=j: e.dma_start(out=h2s[j % NH], in_=h2buf_d[128 * j:128 * (j + 1), :]),
                 reads=[h2bufB[j]], writes=[h2sB[j % NH]], dma=s_h2s[j % NH])
            for idx in (idx1, idx2):
                P.op("gpsimd", lambda e, j=j, idx=idx: e.indirect_dma_start(
                    out=xbuf_d, out_offset=bass.IndirectOffsetOnAxis(ap=idx[:, j:j + 1], axis=0),
                    in_=h2s[j % NH], in_offset=None, bounds_check=bc_reg(e), oob_is_err=False),
                    reads=[h2sB[j % NH], routeB], pwrites=[xbufB], dma=s_scat[j % NH])

        s_w = [P.dsem("s_w%d" % i) for i in range(NW)]
        s_xe = [[P.dsem("s_xe%d_%d" % (i, s)) for s in range(NS)] for i in range(2)]
        s_ye = [P.dsem("s_ye%d" % i) for i in range(3)]
        for ex in range(NE):
            w = ex % NW
            s2 = ex % 2
            wdep = [routeB, sidxB] if w == NW - 1 else []
            P.op("gpsimd", lambda e, ex=ex, w=w: e.dma_start(
                out=wg_bf[w].rearrange("p k f -> p (k f)"), in_=wg_d[ex].rearrange("(p k) f -> p (k f)", p=128),
                max_dma_last_dim=8192), reads=wdep, pwrites=[wslotB[w]], dma=s_w[w])
            P.op("gpsimd", lambda e, ex=ex, w=w: e.dma_start(
                out=wu_bf[w].rearrange("p k f -> p (k f)"), in_=wu_d[ex].rearrange("(p k) f -> p (k f)", p=128),
                max_dma_last_dim=8192), reads=wdep, pwrites=[wslotB[w]], dma=s_w[w])
            P.op("gpsimd", lambda e, ex=ex, w=w: e.dma_start(
                out=wd_bf[w], in_=wd_d[ex].rearrange("(k p) f -> p k f", p=128)), reads=wdep, pwrites=[wslotB[w]], dma=s_w[w])
            for st in range(NS):
                P.op("gpsimd", lambda e, ex=ex, s2=s2, st=st: e.indirect_dma_start(
                    out=xe[s2][:, st, :], out_offset=None, in_=xbuf_d,
                    in_offset=bass.IndirectOffsetOnAxis(ap=sidx_i[:, ex, st:st + 1], axis=0),
                    bounds_check=bc_reg(e), oob_is_err=False),
                    reads=[xbufB, sidxB, routeB], writes=[xeB[s2][st]], dma=s_xe[s2][st], bscale=0.34)
            for st in range(NS):
                tb = st % 2
                def tr_strided(e, s2=s2, st=st, tb=tb):
                    for kk in range(KD):
                        i = e.transpose(out=bank_bf(tb, 8)[:, kk, :], in_=xe[s2][:, st, kk:D:KD], identity=ident[:])
                    return i
                P.op("tensor", tr_strided, reads=[xeB[s2][st], identB], writes=[bank[tb]])
                if st % 2 == 0:
                    P.op("scalar", lambda e, s2=s2, st=st, tb=tb: e.copy(out=xeT[s2][:, :, 128 * st:128 * (st + 1)], in_=bank_bf(tb, 8)),
                         reads=[bank[tb]], writes=[xeTB[s2][st]])
                else:
                    P.op("vector", lambda e, s2=s2, st=st, tb=tb: e.tensor_copy(out=xeT[s2][:, :, 128 * st:128 * (st + 1)], in_=bank_bf(tb, 8)),
                         reads=[bank[tb]], writes=[xeTB[s2][st]])
            for fc in range(4):
                bg = 2 + 2 * (fc % 2)
                bu = bg + 1

                def mm_gu(e, fc=fc, bg=bg, bu=bu, w=w, s2=s2):
                    for bi, wt in ((bg, wg_bf[w]), (bu, wu_bf[w])):
                        for k in range(KD):
                            i = e.matmul(ps[:, bi, 0:CAP], lhsT=wt[:, k, 128 * fc:128 * (fc + 1)],
                                         rhs=xeT[s2][:, k, :], start=(k == 0), stop=(k == KD - 1))
                    return i
                P.op("tensor", mm_gu, reads=[wslotB[w]] + xeTB[s2], writes=[bank[bg], bank[bu]])
                P.op("scalar", lambda e, bg=bg, fc=fc: e.activation(out=sg[fc % 2], in_=ps[:, bg, 0:CAP], func=AF.Silu),
                     reads=[bank[bg]], writes=[sgB[fc % 2]])
                P.op("vector", lambda e, bu=bu, fc=fc, s2=s2: e.tensor_tensor(
                    out=hid[s2][:, fc, :], in0=sg[fc % 2], in1=ps[:, bu, 0:CAP], op=ALU.mult),
                    reads=[sgB[fc % 2], bank[bu]], writes=[hidB[s2][fc]])
            for st in range(NS):
                yi = (ex * NS + st) % 3

                def mm_down(e, st=st, w=w, s2=s2):
                    for dh in range(2):
                        for fc in range(4):
                            i = e.matmul(ps[:, 6 + dh, :], lhsT=hid[s2][:, fc, 128 * st:128 * (st + 1)],
                                         rhs=wd_bf[w][:, fc, 512 * dh:512 * (dh + 1)], start=(fc == 0), stop=(fc == 3))
                    return i
                P.op("tensor", mm_down, reads=hidB[s2] + [wslotB[w]], writes=[bank[6], bank[7]])
                src = ps[:, 6:8, :].rearrange("p a b -> p (a b)")
                if (ex * NS + st) % 2 == 0:
                    P.op("scalar", lambda e, yi=yi, src=src: e.copy(out=ye[yi], in_=src),
                         reads=[bank[6], bank[7]], writes=[yeB[yi]])
                else:
                    P.op("vector", lambda e, yi=yi, src=src: e.tensor_copy(out=ye[yi], in_=src),
                         reads=[bank[6], bank[7]], writes=[yeB[yi]])
                P.op("gpsimd", lambda e, ex=ex, st=st, yi=yi: e.indirect_dma_start(
                    out=ybuf_d, out_offset=bass.IndirectOffsetOnAxis(ap=sidx_i[:, ex, st:st + 1], axis=0),
                    in_=ye[yi], in_offset=None, bounds_check=bc_reg(e), oob_is_err=False),
                    reads=[yeB[yi], sidxB], pwrites=[ybufB], dma=s_ye[yi], bscale=0.34)

        P.barrier()
        apos[0] = 0
        NY = 6
        NP3 = 3
        y1 = [carve([128, D], F32) for _ in range(NY)]
        y2 = [carve([128, D], F32) for _ in range(NY)]
        y1B = [Buf("y1_%d" % i) for i in range(NY)]
        y2B = [Buf("y2_%d" % i) for i in range(NY)]
        h3t = [carve([128, D], BF16) for _ in range(NP3)]
        h3tB = [Buf("h3t%d" % i) for i in range(NP3)]
        h3T = [carve([128, KD, 128], BF16) for _ in range(NP3)]
        h3TB = [Buf("h3T%d" % i) for i in range(NP3)]
        pT = [carve([128, 2, 128], BF16) for _ in range(NP3)]
        pTB = [Buf("pT%d" % i) for i in range(NP3)]
        sgm = [carve([128, D], F32) for _ in range(NP3)]
        sgmB = [Buf("sgm%d" % i) for i in range(NP3)]
        tmul = [carve([128, D], F32) for _ in range(NP3)]
        tmulB = [Buf("tmul%d" % i) for i in range(NP3)]
        ot = [carve([128, D], F32) for _ in range(NP3)]
        otB = [Buf("ot%d" % i) for i in range(NP3)]
        assert apos[0] <= ARENA
        ple_w0 = apos[0]
        wpg_bf = carve([128, KD, D], BF16)
        wpp_bf = carve([128, 2, D], BF16)
        p_bf = carve([128, NT, 256], BF16)
        assert apos[0] <= ARENA, "PLE weight prefetch region overflows the arena: %d" % apos[0]
        wpB, pbB = Buf("wple"), Buf("pbf")
        s_p3 = [P.dsem("s_p3_%d" % i) for i in range(4)]
        def ld_gple(e):
            with nc.allow_non_contiguous_dma(reason="gain vector as per-partition columns"):
                for k in range(KD):
                    i = e.dma_start(out=gplec[:, k:k + 1], in_=gple_d[0:1, 128 * k:128 * (k + 1)].rearrange("o p -> p o"))
                    if k < KD - 1:
                        i.then_inc(s_p3[0].h, 16)
            return i
        s_p3[0].n += 16 * (KD - 1)
        P.op("sync", ld_gple, writes=[gplecB], dma=s_p3[0])
        P.op("sync", lambda e: e.dma_start(out=gB_[:], in_=gfin_d.partition_broadcast(128)), writes=[gBB], dma=s_p3[1])
        P.op("gpsimd", lambda e: e.dma_start(out=wpg_bf, in_=wpg_d.rearrange("(k p) f -> p k f", p=128)),
             pwrites=[wpB], dma=s_p3[2])
        P.op("gpsimd", lambda e: e.dma_start(out=wpp_bf, in_=wpp_d.rearrange("(k p) f -> p k f", p=128)),
             pwrites=[wpB], dma=s_p3[2])
        P.op("gpsimd", lambda e: e.dma_start(out=p_bf, in_=p_d.rearrange("(j p) f -> p j f", p=128)),
             writes=[pbB], dma=s_p3[3])
        for k in range(KD):
            P.op("scalar", lambda e, k=k: e.activation(out=wpg_bf[:, k, :], in_=wpg_bf[:, k, :], func=AF.Copy, scale=gplec[:, k:k + 1]),
                 reads=[gplecB], writes=[wpB])


        s_y1 = [P.dsem("s_y1_%d" % i) for i in range(NY)]
        s_y2 = [P.dsem("s_y2_%d" % i) for i in range(NY)]
        s_out = [P.dsem("s_out%d" % i) for i in range(NP3)]
        out_ops = {}
        for j in range(NT):
            s2 = j % NP3
            b2 = j % 2
            sy = j % NY
            P.op("gpsimd", lambda e, j=j, sy=sy: e.indirect_dma_start(
                out=y1[sy], out_offset=None, in_=ybuf_d,
                in_offset=bass.IndirectOffsetOnAxis(ap=idx1[:, j:j + 1], axis=0),
                bounds_check=bc_reg(e), oob_is_err=False), reads=[ybufB, routeB], writes=[y1B[sy]], dma=s_y1[sy])
            P.op("gpsimd", lambda e, j=j, sy=sy: e.indirect_dma_start(
                out=y2[sy], out_offset=None, in_=ybuf_d,
                in_offset=bass.IndirectOffsetOnAxis(ap=idx2[:, j:j + 1], axis=0),
                bounds_check=bc_reg(e), oob_is_err=False), reads=[ybufB, routeB], writes=[y2B[sy]], dma=s_y2[sy])

            P.op("vector", lambda e, j=j, sy=sy: e.scalar_tensor_tensor(
                out=xres[:, j, :], in0=y1[sy], scalar=w1g[:, j:j + 1], in1=xres[:, j, :], op0=ALU.mult, op1=ALU.add),
                reads=[y1B[sy], routeB], writes=[xresB[j]])
            P.op("vector", lambda e, j=j, sy=sy: e.scalar_tensor_tensor(
                out=xres[:, j, :], in0=y2[sy], scalar=w2g[:, j:j + 1], in1=xres[:, j, :], op0=ALU.mult, op1=ALU.add),
                reads=[y2B[sy], routeB], writes=[xresB[j]])
            if DEBUG:
                P.op("sync", lambda e, j=j: e.dma_start(out=dbg_x3[128 * j:128 * (j + 1), :], in_=xres[:, j, :]),
                     reads=[xresB[j]], dma=s_dbg)
            P.op("scalar", lambda e, j=j, s2=s2: e.activation(out=h3t[s2], in_=xres[:, j, :], func=AF.Square,
                                                              accum_out=ss[:, 0, j:j + 1]),
                 reads=[xresB[j]], writes=[h3tB[s2], ssb(0, j)])
            rstd2(ss[:, 0, j:j + 1], ss[:, 1, j:j + 1], D, ssb(0, j), ssb(1, j))
            P.op("scalar", lambda e, j=j, s2=s2: e.activation(out=h3t[s2], in_=xres[:, j, :], func=AF.Copy, scale=ss[:, 1, j:j + 1]),
                 reads=[xresB[j], ssb(1, j)], writes=[h3tB[s2]])
            P.op("tensor", tr_generic(h3t[s2], b2, 8), reads=[h3tB[s2], identB], writes=[bank[b2]])
            P.op("scalar", lambda e, s2=s2, b2=b2: e.copy(out=h3T[s2], in_=bank_bf(b2, 8)), reads=[bank[b2]], writes=[h3TB[s2]])
            P.op("tensor", tr_generic(p_bf[:, j, :], 6 + b2, 2), reads=[pbB, identB], writes=[bank[6 + b2]])
            P.op("vector", lambda e, s2=s2, b2=b2: e.tensor_copy(out=pT[s2], in_=bank_bf(6 + b2, 2)), reads=[bank[6 + b2]], writes=[pTB[s2]])

            def mm_pg(e, s2=s2):
                for half in range(2):
                    for k in range(KD):
                        i = e.matmul(ps[:, 2 + half, :], lhsT=h3T[s2][:, k, :], rhs=wpg_bf[:, k, 512 * half:512 * (half + 1)],
                                     start=(k == 0), stop=(k == KD - 1))
                return i

            def mm_pp(e, s2=s2):
                for half in range(2):
                    for k in range(2):
                        i = e.matmul(ps[:, 4 + half, :], lhsT=pT[s2][:, k, :], rhs=wpp_bf[:, k, 512 * half:512 * (half + 1)],
                                     start=(k == 0), stop=(k == 1))
                return i
            P.op("tensor", mm_pg, reads=[h3TB[s2], wpB], writes=[bank[2], bank[3]])
            P.op("tensor", mm_pp, reads=[pTB[s2], wpB], writes=[bank[4], bank[5]])
            P.op("scalar", lambda e, s2=s2: e.activation(out=sgm[s2], in_=ps[:, 2:4, :].rearrange("p a b -> p (a b)"), func=AF.Sigmoid),
                 reads=[bank[2], bank[3]], writes=[sgmB[s2]])
            P.op("vector", lambda e, s2=s2: e.tensor_tensor(out=tmul[s2], in0=sgm[s2], in1=ps[:, 4:6, :].rearrange("p a b -> p (a b)"),
                                                            op=ALU.mult), reads=[sgmB[s2], bank[4], bank[5]], writes=[tmulB[s2]])
            P.op("gpsimd", lambda e, j=j, s2=s2: e.tensor_tensor(out=xres[:, j, :], in0=xres[:, j, :], in1=tmul[s2], op=ALU.add),
                 reads=[tmulB[s2]], writes=[xresB[j]])
            P.op("scalar", lambda e, j=j, s2=s2: e.activation(out=ot[s2], in_=xres[:, j, :], func=AF.Square,
                                                              accum_out=ss[:, 2, j:j + 1]),
                 reads=[xresB[j]], writes=[otB[s2], ssb(2, j)])
            rstd2(ss[:, 2, j:j + 1], ss[:, 3, j:j + 1], D, ssb(2, j), ssb(3, j))
            P.op("vector", lambda e, j=j, s2=s2: e.scalar_tensor_tensor(
                out=ot[s2], in0=xres[:, j, :], scalar=ss[:, 3, j:j + 1], in1=gB_[:], op0=ALU.mult, op1=ALU.mult),
                reads=[xresB[j], ssb(3, j), gBB], writes=[otB[s2]])
            out_ops[s2] = P.op("sync", lambda e, j=j, s2=s2: e.dma_start(out=out_d[128 * j:128 * (j + 1), :], in_=ot[s2]),
                               reads=[otB[s2]], dma=s_out[s2])
        P.wait_all("sync", list(out_ops.values()))

        block = es.enter_context(nc.Block())
        P.emit(block)
        build_nc.makespan = P.makespan
    return nc


_NC_CACHE = {}


def kernel(x, p, g_mix, w_in, w_conv, g_sgu, w_spatial, b_spatial, w_out, g_ffn,
           w_group, b_group, w_router, b_router, w_gate, w_up, w_down,
           g_ple, w_ple_gate, w_ple_proj, g_final):
    f = lambda a: np.ascontiguousarray(np.asarray(a, dtype=np.float32))
    x = f(x)
    p = f(p)
    shared = {
        "g_mix": f(g_mix).reshape(1, D),
        "w_in": f(w_in)[0],
        "w_conv": f(w_conv)[0],
        "g_sgu": f(g_sgu).reshape(1, 512),
        "w_spatial": f(w_spatial)[0],
        "b_spatial": f(b_spatial)[0],
        "w_out": f(w_out)[0],
        "g_ffn": f(g_ffn).reshape(1, D),
        "w_group": f(w_group)[0],
        "b_group": f(b_group).reshape(1, 4),
        "w_router": f(w_router)[0],
        "b_router": f(b_router).reshape(1, 32),
        "w_gate": f(w_gate)[0].reshape(NE, D, FE),
        "w_up": f(w_up)[0].reshape(NE, D, FE),
        "w_down": f(w_down)[0].reshape(NE, FE, D),
        "g_ple": f(g_ple).reshape(1, D),
        "w_ple_gate": f(w_ple_gate)[0],
        "w_ple_proj": f(w_ple_proj)[0],
        "g_final": f(g_final).reshape(1, D),
    }
    in_maps = []
    for c in range(NCORES):
        b, half = c // 2, c % 2
        t0 = half * TOK
        xc = x[b, t0:t0 + TOK]
        if half == 0:
            xh = np.zeros((2, D), np.float32)
        else:
            xh = x[b, t0 - 2:t0]
        m = dict(shared)
        m["x"] = np.ascontiguousarray(xc)
        m["xh"] = np.ascontiguousarray(xh)
        m["p"] = np.ascontiguousarray(p[0, b, t0:t0 + TOK])
        in_maps.append(m)
    if "nc" not in _NC_CACHE:
        _NC_CACHE["nc"] = build_nc()
    res = run_bass_kernel_spmd(_NC_CACHE["nc"], in_maps, core_ids=list(range(NCORES)))
    out = np.empty((4, 4096, D), np.float32)
    for c in range(NCORES):
        b, half = c // 2, c % 2
        out[b, half * TOK:(half + 1) * TOK] = res.results[c]["out"]
    return out
```
